# Optimizing a Trainium2 kernel written in Bass

```python
import jax
import jax.numpy as jnp
from jax import lax
import numpy as np

D_MODEL = 1024
BATCH = 4
SEQ = 4096
DEPTH = 2

CHUNK = 64
QBLOCK = 128
ROPE_THETA = 500000.0
NORM_EPS = 1e-6

SB_HEADS = 8
SB_HEAD_DIM = 64
SGU_GROUPS = 8
SGU_GROUP_DIM = 64
SGU_CHUNK = 128
AB_SPLITS = (SB_HEADS * SB_HEAD_DIM, SB_HEADS * SB_HEAD_DIM, SB_HEADS * SB_HEAD_DIM,
             SGU_GROUPS * SGU_GROUP_DIM, SGU_GROUPS * SGU_GROUP_DIM)
AB_COLS = sum(AB_SPLITS)
AB_MIX = SB_HEADS * SB_HEAD_DIM + SGU_GROUPS * SGU_GROUP_DIM

MLA_HEADS = 8
MLA_Q_RANK = 256
MLA_KV_RANK = 128
MLA_ROPE_DIM = 32
MLA_NOPE_DIM = 64
MLA_V_DIM = 64
DSA_HEADS = 8
DSA_HEAD_DIM = 64
DSA_ROT_DIM = DSA_HEAD_DIM // 4
IDX_HEADS = 8
IDX_HEAD_DIM = 32
IDX_ROT_DIM = IDX_HEAD_DIM // 4
INDEX_TOPK = 256
CD_SPLITS = (MLA_Q_RANK, MLA_KV_RANK, MLA_ROPE_DIM,
             DSA_HEADS * DSA_HEAD_DIM, DSA_HEADS * DSA_HEAD_DIM, DSA_HEADS * DSA_HEAD_DIM,
             IDX_HEADS * IDX_HEAD_DIM, IDX_HEAD_DIM, IDX_HEADS)
CD_COLS = sum(CD_SPLITS)
CD_MIX = MLA_HEADS * MLA_V_DIM + DSA_HEADS * DSA_HEAD_DIM

MOE_GROUPS = 4
EXPERTS_PER_GROUP = 4
N_EXPERTS = MOE_GROUPS * EXPERTS_PER_GROUP
EXPERT_TOPK = 2
D_EXPERT = 512

N_EVEN_LAYERS = (DEPTH + 1) // 2
N_ODD_LAYERS = DEPTH // 2

kernel_name = 'hybrid_stickbreak_sgu_mla_dsa_hmoe'


def rms_norm(x, g):
    x32 = x.astype(jnp.float32)
    y = x32 * lax.rsqrt(jnp.mean(x32 * x32, axis=-1, keepdims=True) + NORM_EPS)
    return (y * g.astype(jnp.float32)).astype(x.dtype)


def rotary(x, positions, rot_dim):
    half = rot_dim // 2
    inv_freq = jnp.power(jnp.float32(ROPE_THETA), -jnp.arange(half, dtype=jnp.float32) * (2.0 / rot_dim))
    ang = positions.astype(jnp.float32)[:, :, None, None] * inv_freq
    cos, sin = jnp.cos(ang), jnp.sin(ang)
    x32 = x.astype(jnp.float32)
    x1, x2 = x32[..., :half], x32[..., half:rot_dim]
    out = jnp.concatenate([x1 * cos - x2 * sin, x2 * cos + x1 * sin, x32[..., rot_dim:]], axis=-1)
    return out.astype(x.dtype)


def split_cols(t, sizes):
    return jnp.split(t, [int(c) for c in np.cumsum(sizes)[:-1]], axis=-1)


def to_blocks(t):
    b, s = t.shape[0], t.shape[1]
    return jnp.swapaxes(t.reshape((b, s // QBLOCK, QBLOCK) + t.shape[2:]), 0, 1)


def from_blocks(t):
    nb, b = t.shape[0], t.shape[1]
    t = jnp.swapaxes(t, 0, 1)
    return t.reshape((b, nb * QBLOCK) + t.shape[3:])


def stick_breaking_attention(q, k, v):
    s_len, d = q.shape[1], q.shape[-1]
    scale = d ** -0.5
    key_pos = jnp.arange(s_len)

    def one_block(args):
        q_blk, blk = args
        q_pos = blk * QBLOCK + jnp.arange(QBLOCK)
        strict = key_pos[None, :] < q_pos[:, None]
        z = jnp.einsum('bqhd,bshd->bhqs', q_blk, k).astype(jnp.float32) * scale
        log_not = jnp.where(strict, -jax.nn.softplus(z), 0.0)
        later = lax.cumsum(log_not, axis=3, reverse=True) - log_not
        w = jnp.where(strict, jnp.exp(jax.nn.log_sigmoid(z) + later), 0.0)
        return jnp.einsum('bhqs,bshd->bqhd', w.astype(v.dtype), v)

    return from_blocks(lax.map(one_block, (to_blocks(q), jnp.arange(s_len // QBLOCK))))


def spatial_gating(u, z, norm_g, w_s, b_s):
    b, s_len, g, dg = z.shape
    z = rms_norm(z, norm_g)
    zc = z.reshape(b, s_len // SGU_CHUNK, SGU_CHUNK, g, dg)
    i = jnp.arange(SGU_CHUNK) // CHUNK
    visible = i[None, :] <= i[:, None]
    w = jnp.where(visible[None], w_s, 0.0).astype(z.dtype)
    mixed = jnp.einsum('gij,bnjgd->bnigd', w, zc) + jnp.swapaxes(b_s, 0, 1)[None, None, :, :, None]
    return u * mixed.reshape(b, s_len, g, dg)


def chunk_causal_attention(q, k, v):
    s_len = q.shape[1]
    scale = q.shape[-1] ** -0.5
    key_chunk = jnp.arange(s_len) // CHUNK

    def one_block(args):
        q_blk, blk = args
        q_chunk = (blk * QBLOCK + jnp.arange(QBLOCK)) // CHUNK
        visible = key_chunk[None, :] <= q_chunk[:, None]
        sc = jnp.einsum('bqhd,bshd->bhqs', q_blk, k).astype(jnp.float32) * scale
        p = jax.nn.softmax(jnp.where(visible, sc, -jnp.inf), axis=-1)
        return jnp.einsum('bhqs,bshd->bqhd', p.astype(v.dtype), v)

    return from_blocks(lax.map(one_block, (to_blocks(q), jnp.arange(s_len // QBLOCK))))


def indexed_sparse_attention(q, k, v, iq, ik, iw, topk):
    s_len = q.shape[1]
    scale = q.shape[-1] ** -0.5
    idx_scale = iq.shape[-1] ** -0.5
    key_chunk = jnp.arange(s_len) // CHUNK

    def one_block(args):
        q_blk, iq_blk, iw_blk, blk = args
        q_chunk = (blk * QBLOCK + jnp.arange(QBLOCK)) // CHUNK
        admissible = key_chunk[None, :] <= q_chunk[:, None]
        logits = jnp.einsum('bqhe,bse->bqhs', iq_blk, ik).astype(jnp.float32) * idx_scale
        score = jnp.einsum('bqh,bqhs->bqs', iw_blk.astype(jnp.float32), jax.nn.relu(logits))
        score = jnp.where(admissible[None], score, -jnp.inf)
        top_score, top_idx = lax.top_k(score, topk)
        valid = top_score > -jnp.inf
        k_sel = jax.vmap(lambda kb, ib: kb[ib])(k, top_idx)
        v_sel = jax.vmap(lambda vb, ib: vb[ib])(v, top_idx)
        sc = jnp.einsum('bqhd,bqkhd->bhqk', q_blk, k_sel).astype(jnp.float32) * scale
        p = jax.nn.softmax(jnp.where(valid[:, None], sc, -jnp.inf), axis=-1)
        return jnp.einsum('bhqk,bqkhd->bqhd', p.astype(v.dtype), v_sel)

    blocks = (to_blocks(q), to_blocks(iq), to_blocks(iw), jnp.arange(s_len // QBLOCK))
    return from_blocks(lax.map(one_block, blocks))


def even_mixer(h, w_in, sb_q_g, sb_k_g, sgu_g, w_s, b_s, w_out):
    b, s_len, _ = h.shape
    q, k, v, u, z = split_cols(h @ w_in, AB_SPLITS)
    hshape = (b, s_len, SB_HEADS, SB_HEAD_DIM)
    gshape = (b, s_len, SGU_GROUPS, SGU_GROUP_DIM)
    a_out = stick_breaking_attention(rms_norm(q.reshape(hshape), sb_q_g),
                                     rms_norm(k.reshape(hshape), sb_k_g), v.reshape(hshape))
    b_out = spatial_gating(jax.nn.gelu(u).reshape(gshape), jax.nn.gelu(z).reshape(gshape), sgu_g, w_s, b_s)
    mixed = jnp.concatenate([a_out.reshape(b, s_len, -1), b_out.reshape(b, s_len, -1)], axis=-1)
    return mixed @ w_out


def odd_mixer(h, positions, w_in, q_lat_g, kv_lat_g, w_uq, w_ukv, mla_q_g, mla_kn_g, mla_kr_g,
              dsa_q_g, dsa_k_g, w_out, topk):
    b, s_len, _ = h.shape
    c_q, c_kv, k_pe, q_d, k_d, v_d, iq, ik, iw = split_cols(h @ w_in, CD_SPLITS)
    q_c = (rms_norm(c_q, q_lat_g) @ w_uq).reshape(b, s_len, MLA_HEADS, MLA_ROPE_DIM + MLA_NOPE_DIM)
    kv_c = (rms_norm(c_kv, kv_lat_g) @ w_ukv).reshape(b, s_len, MLA_HEADS, MLA_NOPE_DIM + MLA_V_DIM)
    k_nope, v_c = kv_c[..., :MLA_NOPE_DIM], kv_c[..., MLA_NOPE_DIM:]
    q_c = rotary(rms_norm(q_c, mla_q_g), positions, MLA_ROPE_DIM)
    k_rope = rotary(rms_norm(k_pe, mla_kr_g)[:, :, None, :], positions, MLA_ROPE_DIM)
    k_c = jnp.concatenate([jnp.broadcast_to(k_rope, (b, s_len, MLA_HEADS, MLA_ROPE_DIM)),
                           rms_norm(k_nope, mla_kn_g)], axis=-1)
    c_out = chunk_causal_attention(q_c, k_c, v_c)
    dshape = (b, s_len, DSA_HEADS, DSA_HEAD_DIM)
    q_d = rotary(rms_norm(q_d.reshape(dshape), dsa_q_g), positions, DSA_ROT_DIM)
    k_d = rotary(rms_norm(k_d.reshape(dshape), dsa_k_g), positions, DSA_ROT_DIM)
    iq = rotary(iq.reshape(b, s_len, IDX_HEADS, IDX_HEAD_DIM), positions, IDX_ROT_DIM)
    ik = rotary(ik[:, :, None, :], positions, IDX_ROT_DIM)[:, :, 0]
    iw = iw * (IDX_HEADS ** -0.5)
    d_out = indexed_sparse_attention(q_d, k_d, v_d.reshape(dshape), iq, ik, iw, topk)
    mixed = jnp.concatenate([c_out.reshape(b, s_len, -1), d_out.reshape(b, s_len, -1)], axis=-1)
    return mixed @ w_out


def hierarchical_moe(h, w_group, b_group, w_expert, b_expert, w_gate, w_up, w_down):
    b, s_len, d = h.shape
    t = h.reshape(b * s_len, d)
    f32 = jnp.float32
    group_probs = jax.nn.softmax((t @ w_group).astype(f32) + b_group.astype(f32), axis=-1)
    p_group, g_idx = lax.top_k(group_probs, 1)
    expert_logits = jnp.einsum('td,gde->tge', t, w_expert).astype(f32) + b_expert.astype(f32)
    in_group = jnp.einsum('tge,tg->te', expert_logits, jax.nn.one_hot(g_idx[:, 0], MOE_GROUPS, dtype=f32))
    p_in, e_idx = lax.top_k(jax.nn.softmax(in_group, axis=-1), EXPERT_TOPK)
    combine = p_group * p_in / jnp.sum(p_in, axis=-1, keepdims=True)
    expert_id = g_idx * EXPERTS_PER_GROUP + e_idx
    gates = jnp.einsum('tk,tke->te', combine, jax.nn.one_hot(expert_id, N_EXPERTS, dtype=f32)).astype(h.dtype)
    y = jnp.zeros_like(t)
    for e in range(N_EXPERTS):
        hid = jax.nn.silu(t @ w_gate[e]) * (t @ w_up[e])
        y = y + (hid @ w_down[e]) * gates[:, e:e + 1]
    return y.reshape(b, s_len, d)


def setup_inputs(seed: int = 0) -> dict:
    key = jax.random.key(seed)
    keys = list(jax.random.split(key, 40))
    f32 = jnp.float32

    def normal(shape, scale):
        return jax.random.normal(keys.pop(), shape, f32) * scale

    def gain(shape):
        return 1.0 + 0.02 * jax.random.normal(keys.pop(), shape, f32)

    ne, no = N_EVEN_LAYERS, N_ODD_LAYERS
    x = normal((BATCH, SEQ, D_MODEL), 1.0)
    offset = jax.random.randint(keys.pop(), (BATCH, 1), 0, 256) * CHUNK
    positions = (offset + jnp.arange(SEQ)[None, :]).astype(jnp.int32)
    return {
        'x': x,
        'positions': positions,
        'ab_norm_g': gain((ne, D_MODEL)),
        'ab_w_in': normal((ne, D_MODEL, AB_COLS), D_MODEL ** -0.5),
        'sb_q_norm_g': gain((ne, SB_HEAD_DIM)),
        'sb_k_norm_g': gain((ne, SB_HEAD_DIM)),
        'sgu_norm_g': gain((ne, SGU_GROUPS, SGU_GROUP_DIM)),
        'sgu_w_s': normal((ne, SGU_GROUPS, SGU_CHUNK, SGU_CHUNK), SGU_CHUNK ** -0.5),
        'sgu_b_s': gain((ne, SGU_GROUPS, SGU_CHUNK)),
        'ab_w_out': normal((ne, AB_MIX, D_MODEL), AB_MIX ** -0.5),
        'cd_norm_g': gain((no, D_MODEL)),
        'cd_w_in': normal((no, D_MODEL, CD_COLS), D_MODEL ** -0.5),
        'mla_q_latent_norm_g': gain((no, MLA_Q_RANK)),
        'mla_kv_latent_norm_g': gain((no, MLA_KV_RANK)),
        'mla_w_uq': normal((no, MLA_Q_RANK, MLA_HEADS * (MLA_ROPE_DIM + MLA_NOPE_DIM)), MLA_Q_RANK ** -0.5),
        'mla_w_ukv': normal((no, MLA_KV_RANK, MLA_HEADS * (MLA_NOPE_DIM + MLA_V_DIM)), MLA_KV_RANK ** -0.5),
        'mla_q_norm_g': gain((no, MLA_ROPE_DIM + MLA_NOPE_DIM)),
        'mla_k_nope_norm_g': gain((no, MLA_NOPE_DIM)),
        'mla_k_rope_norm_g': gain((no, MLA_ROPE_DIM)),
        'dsa_q_norm_g': gain((no, DSA_HEAD_DIM)),
        'dsa_k_norm_g': gain((no, DSA_HEAD_DIM)),
        'cd_w_out': normal((no, CD_MIX, D_MODEL), CD_MIX ** -0.5),
        'ffn_norm_g': gain((DEPTH, D_MODEL)),
        'router_group_w': normal((DEPTH, D_MODEL, MOE_GROUPS), D_MODEL ** -0.5),
        'router_group_b': normal((DEPTH, MOE_GROUPS), 0.01),
        'router_expert_w': normal((DEPTH, MOE_GROUPS, D_MODEL, EXPERTS_PER_GROUP), D_MODEL ** -0.5),
        'router_expert_b': normal((DEPTH, MOE_GROUPS, EXPERTS_PER_GROUP), 0.01),
        'expert_w_gate': normal((DEPTH, N_EXPERTS, D_MODEL, D_EXPERT), D_MODEL ** -0.5),
        'expert_w_up': normal((DEPTH, N_EXPERTS, D_MODEL, D_EXPERT), D_MODEL ** -0.5),
        'expert_w_down': normal((DEPTH, N_EXPERTS, D_EXPERT, D_MODEL), D_EXPERT ** -0.5),
    }


def reference(x, positions, ab_norm_g, ab_w_in, sb_q_norm_g, sb_k_norm_g, sgu_norm_g, sgu_w_s, sgu_b_s,
              ab_w_out, cd_norm_g, cd_w_in, mla_q_latent_norm_g, mla_kv_latent_norm_g, mla_w_uq, mla_w_ukv,
              mla_q_norm_g, mla_k_nope_norm_g, mla_k_rope_norm_g, dsa_q_norm_g, dsa_k_norm_g, cd_w_out,
              ffn_norm_g, router_group_w, router_group_b, router_expert_w, router_expert_b,
              expert_w_gate, expert_w_up, expert_w_down):
    topk = min(INDEX_TOPK, x.shape[1] // 4)
    h = x
    for layer in range(DEPTH):
        i = layer // 2
        if layer % 2 == 0:
            h = h + even_mixer(rms_norm(h, ab_norm_g[i]), ab_w_in[i], sb_q_norm_g[i], sb_k_norm_g[i],
                               sgu_norm_g[i], sgu_w_s[i], sgu_b_s[i], ab_w_out[i])
        else:
            h = h + odd_mixer(rms_norm(h, cd_norm_g[i]), positions, cd_w_in[i], mla_q_latent_norm_g[i],
                              mla_kv_latent_norm_g[i], mla_w_uq[i], mla_w_ukv[i], mla_q_norm_g[i],
                              mla_k_nope_norm_g[i], mla_k_rope_norm_g[i], dsa_q_norm_g[i], dsa_k_norm_g[i],
                              cd_w_out[i], topk)
        h = h + hierarchical_moe(rms_norm(h, ffn_norm_g[layer]), router_group_w[layer], router_group_b[layer],
                                 router_expert_w[layer], router_expert_b[layer], expert_w_gate[layer],
                                 expert_w_up[layer], expert_w_down[layer])
    return h
```

```python
import math
from contextlib import ExitStack
import numpy as np
import concourse.bass as bass
import concourse.mybir as mybir
from concourse.bass_utils import run_bass_kernel_spmd

F32 = mybir.dt.float32
BF16 = mybir.dt.bfloat16
I32 = mybir.dt.int32
AF = mybir.ActivationFunctionType
ALU = mybir.AluOpType
AX = mybir.AxisListType

EPOCH = 20000
D_MODEL = 1024
NEG = -30000.0
NBIS = 24
NOINT = False
TWO_PI = 2.0 * math.pi
CW1 = 6.28125
CW2 = 0.00193500518798828125
CW3 = TWO_PI - CW1 - CW2


class Dep:
    __slots__ = ("w", "rs")

    def __init__(self):
        self.w = None
        self.rs = []


class T:
    def __init__(self, t):
        self.t = t
        self.d = Dep()

    def __getitem__(self, k):
        return self.t[k]


class Op:
    __slots__ = ("eng", "fn", "waits", "idx", "chan", "cnt", "is_dma")


def _dep(x):
    return x.d if isinstance(x, T) else x


class Prog:
    ENGS = ("pe", "act", "dve", "pool", "sp")

    def __init__(self, nc, n_chan=40):
        self.nc = nc
        self.ops = {e: [] for e in self.ENGS}
        self.nidx = {e: 0 for e in self.ENGS}
        self.known = {e: {} for e in self.ENGS}
        self.chan_cnt = [0] * n_chan
        self.n_chan = n_chan
        self.rr = 0
        self.rr_sw = 0
        self.n_sw = 8
        self.pending = {e: [] for e in self.ENGS}

    def _wait(self, op, key, val):
        kn = self.known[op.eng]
        if kn.get(key, 0) >= val:
            return
        kn[key] = val
        op.waits = [w for w in op.waits if w[0] != key]
        op.waits.append((key, val))

    def _need(self, op, src):
        if src is None:
            return
        if src.is_dma:
            self._wait(op, ("c", src.chan), src.cnt)
        else:
            if src.eng == op.eng and op.eng == "pe":
                return
            self._wait(op, ("e", src.eng), src.idx + 1)

    def op(self, eng, fn, r=(), w=(), dma=False):
        o = Op()
        o.eng = eng
        o.fn = fn
        o.waits = []
        o.is_dma = dma
        for key, val in self.pending[eng]:
            self._wait(o, key, val)
        self.pending[eng] = []
        if dma:
            if eng == "pool":
                ch = self.n_chan - self.n_sw + self.rr_sw
                self.rr_sw = (self.rr_sw + 1) % self.n_sw
            else:
                ch = self.rr
                self.rr = (self.rr + 1) % (self.n_chan - self.n_sw)
            o.chan = ch
            if self.chan_cnt[ch] > 0:
                self._wait(o, ("c", ch), self.chan_cnt[ch])
            self.chan_cnt[ch] += 16
            o.cnt = self.chan_cnt[ch]
            o.idx = None
        else:
            o.chan = None
            o.cnt = None
            o.idx = self.nidx[eng]
            self.nidx[eng] += 1
        r = [_dep(x) for x in r]
        w = [_dep(x) for x in w]
        for d in r:
            self._need(o, d.w)
        for d in w:
            self._need(o, d.w)
            for q in d.rs:
                self._need(o, q)
        for d in r:
            d.rs.append(o)
        for d in w:
            d.w = o
            d.rs = []
        self.ops[eng].append(o)
        return o

    def barrier(self):
        waits = []
        for e in ("pe", "act", "dve", "pool"):
            if self.nidx[e] > 0:
                waits.append((("e", e), self.nidx[e]))
        for c in range(self.n_chan):
            if self.chan_cnt[c] > 0:
                waits.append((("c", c), self.chan_cnt[c]))
        for e in self.ENGS:
            self.pending[e] = list(waits)

    def dma(self, q, out, in_, r=(), w=()):
        return self.op(q, lambda e: e.dma_start(out=out, in_=in_), r, w, dma=True)

    def act(self, out, in_, func, r, w, **kw):
        return self.op("act", lambda e: e.activation(out=out, in_=in_, func=func, **kw), r, w)

    def tt(self, eng, out, in0, in1, op, r, w):
        return self.op(eng, lambda e: e.tensor_tensor(out=out, in0=in0, in1=in1, op=op), r, w)

    def ts(self, eng, out, in0, s1, s2, op0, op1, r, w, accum_out=None):
        if op1 is None:
            return self.op(eng, lambda e: e.tensor_scalar(out=out, in0=in0, scalar1=s1, scalar2=None, op0=op0), r, w)
        return self.op(eng, lambda e: e.tensor_scalar(out=out, in0=in0, scalar1=s1, scalar2=s2, op0=op0, op1=op1,
                                                      accum_out=accum_out), r, w)

    def stt(self, eng, out, in0, scalar, in1, op0, op1, r, w):
        return self.op(eng, lambda e: e.scalar_tensor_tensor(out=out, in0=in0, scalar=scalar, in1=in1, op0=op0, op1=op1), r, w)

    def mm(self, out, lhsT, rhs, start, stop, r, w):
        return self.op("pe", lambda e: e.matmul(out, lhsT=lhsT, rhs=rhs, start=start, stop=stop), r, w)

    def tr(self, out, in_, ident, r, w):
        return self.op("pe", lambda e: e.transpose(out, in_, ident), r, w)

    def copy(self, eng, out, in_, r, w):
        if eng == "act":
            return self.op("act", lambda e: e.copy(out=out, in_=in_), r, w)
        return self.op(eng, lambda e: e.tensor_copy(out=out, in_=in_), r, w)

    def memset(self, eng, ap, val, w):
        return self.op(eng, lambda e: e.memset(ap, val), (), w)

    def reduce(self, out, in_, op, r, w, axis=None):
        ax = AX.X if axis is None else axis
        return self.op("dve", lambda e: e.tensor_reduce(out=out, in_=in_, axis=ax, op=op), r, w)

    def recip(self, out, in_, r, w):
        return self.op("dve", lambda e: e.reciprocal(out=out, in_=in_), r, w)

    def emit(self):
        nc = self.nc
        with ExitStack() as es:
            esem = {e: [es.enter_context(nc.semaphore(f"s_{e}_{i}"))
                        for i in range(max((self.nidx[e] + EPOCH - 1) // EPOCH, 1))]
                    for e in ("pe", "act", "dve", "pool")}
            csem = [es.enter_context(nc.semaphore(f"c_{i}")) for i in range(self.n_chan)]
            block = es.enter_context(nc.Block())

            def do_waits(e, waits):
                for key, val in waits:
                    if key[0] == "c":
                        e.wait_ge(csem[key[1]], val)
                    else:
                        idx = val - 1
                        ep = idx // EPOCH
                        e.wait_ge(esem[key[1]][ep], idx - ep * EPOCH + 1)

            def run(e, name):
                for o in self.ops[name]:
                    do_waits(e, o.waits)
                    ins = o.fn(e)
                    if o.is_dma:
                        ins.then_inc(csem[o.chan], 16)
                    else:
                        ins.then_inc(esem[name][o.idx // EPOCH], 1)

            @block.tensor
            def _(e):
                run(e, "pe")

            @block.scalar
            def _(e):
                run(e, "act")

            @block.vector
            def _(e):
                run(e, "dve")

            @block.gpsimd
            def _(e):
                run(e, "pool")

            @block.sync
            def _(e):
                run(e, "sp")
                fin = []
                for c in range(self.n_chan):
                    if self.chan_cnt[c] > 0:
                        fin.append((("c", c), self.chan_cnt[c]))
                for en in ("pe", "act", "dve", "pool"):
                    if self.nidx[en] > 0:
                        fin.append((("e", en), self.nidx[en]))
                do_waits(e, fin)


class Ring:
    def __init__(self, items):
        self.items = items

    def get(self, i):
        return self.items[i % len(self.items)]


def build(S, stop="full", TOPK=256):
    NT = S // 128
    NG = S // 512
    SBT = min(8, NT)
    NSB = NT // SBT
    nc = bass.Bass("TRN2", target_bir_lowering=False)
    P = Prog(nc)

    def din(name, shape, dt=F32):
        return nc.dram_tensor(name, list(shape), dt, kind="ExternalInput").ap()

    def dscr(name, shape, dt):
        return nc.dram_tensor(name, list(shape), dt, kind="Internal").ap()

    x_d = din("x", [S, 1024])
    pos_d = din("pos", [128, NT], I32)
    c_ident = din("c_ident", [128, 128])
    c_sgumask = din("c_sgumask", [128, 128])
    c_negsb = din("c_negsb", [4, 128, 512])
    c_negcc = din("c_negcc", [4, 128, 512])
    c_adm = din("c_adm", [128, 128])
    c_invf = din("c_invf", [128, 28])
    c_pow2 = din("c_pow2", [128, NBIS + 1])
    c_tri = din("c_tri", [128, 128])
    c_trii = din("c_trii", [128, 128])
    g0col = din("g0col", [128, 8])
    w_in0 = din("w_in0", [1024, 2560])
    gq_d = din("gq", [128, 1])
    gk_d = din("gk", [128, 1])
    sgu_g_d = din("sgu_g", [1, 512])
    sgu_wT_d = din("sgu_wT", [128, 8, 128])
    sgu_b_d = din("sgu_b", [128, 8])
    w_out0 = din("w_out0", [1024, 1024])
    g2col = [din(f"g2col{l}", [128, 8]) for l in range(2)]
    wr_d = [din(f"wr{l}", [1024, 20]) for l in range(2)]
    rb_d = [din(f"rb{l}", [1, 20]) for l in range(2)]
    wg_d = [din(f"wg{l}", [16, 1024, 512]) for l in range(2)]
    wu_d = [din(f"wu{l}", [16, 1024, 512]) for l in range(2)]
    wd_d = [din(f"wd{l}", [16, 512, 1024]) for l in range(2)]
    g1col = din("g1col", [128, 8])
    w_in1 = din("w_in1", [1024, 2304])
    qlat_col = din("qlat_col", [128, 2])
    kvlat_col = din("kvlat_col", [128, 1])
    w_uq_d = din("w_uq", [256, 768])
    w_ukv_d = din("w_ukv", [128, 1024])
    mla_qg_d = din("mla_qg", [1, 96])
    mla_kn_col = din("mla_kn_col", [128, 1])
    mla_kr_d = din("mla_kr", [1, 32])
    dsa_qg_d = din("dsa_qg", [1, 64])
    dsa_kg_d = din("dsa_kg", [1, 64])
    w_out1 = din("w_out1", [1024, 1024])
    out_d = nc.dram_tensor("out", [S, 1024], F32, kind="ExternalOutput").ap()

    qT0_s = dscr("qT0_s", [4, 128, S], BF16)
    kT0_s = dscr("kT0_s", [4, 128, S], BF16)
    v0_s = dscr("v0_s", [S, 512], BF16)
    mixed_s = dscr("mixed_s", [S, 1024], BF16)
    h1_s = dscr("h1_s", [S, 1024], F32)
    qTc_s = dscr("qTc_s", [8, 96, S], BF16)
    kTc_s = dscr("kTc_s", [8, 96, S], BF16)
    vc_s = dscr("vc_s", [S, 512], BF16)
    qTd_s = dscr("qTd_s", [4, 128, S], BF16)
    kTd_s = dscr("kTd_s", [4, 128, S], BF16)
    vd_s = dscr("vd_s", [S, 512], BF16)
    aT_s = dscr("aT_s", [4, 64, S], BF16)
    ikT_s = dscr("ikT_s", [64, S], BF16)
    NMT_d = dscr("NMT_d", [NG, NT, 128, 512], BF16)
    hmid_s = dscr("hmid_s", [S, 1024], F32)

    top = ExitStack()
    with top:
        uniq = [0]

        def sb(es, name, shape, dt=F32):
            uniq[0] += 1
            return T(es.enter_context(nc.sbuf_tensor(f"sb{uniq[0]}_{name}", list(shape), dt)))

        def ps(es, name, shape, dt=F32):
            uniq[0] += 1
            return T(es.enter_context(nc.psum_tensor(f"ps{uniq[0]}_{name}", list(shape), dt)))

        def bc(ap, shape):
            return ap.to_broadcast(list(shape))

        ident_f = sb(top, "ident_f", [128, 128])
        ident_b = sb(top, "ident_b", [128, 128], BF16)
        eps_t = sb(top, "eps_t", [128, 1])
        cosT = sb(top, "cosT", [128, NT, 28])
        sinT = sb(top, "sinT", [128, NT, 28])
        sgnT = sb(top, "sgnT", [128, NT, 8])
        P.dma("sp", ident_f[:], c_ident, w=[ident_f])
        P.dma("pool", ident_b[:], c_ident, w=[ident_b])
        P.memset("dve", eps_t[:], 1e-6, w=[eps_t])

        def rstd_of(es_tag, ss_ap, n_el, width, scr_sd, out_r, r, eng_w):
            P.act(scr_sd[:, 0:width], ss_ap, AF.Sqrt, r=r, w=[scr_sd], scale=1.0 / n_el, bias=eps_t[:, 0:1])
            P.recip(out_r[:, 0:width], scr_sd[:, 0:width], r=[scr_sd], w=[out_r])

        def run_rolling(gen_fn, n_tiles, stagger):
            active = []
            nxt_t = 0
            step = 0
            while active or nxt_t < n_tiles:
                if nxt_t < n_tiles and (not active or (len(active) < 2 and (nxt_t != 1 or step >= stagger))):
                    active.append(gen_fn(nxt_t))
                    nxt_t += 1
                for gg in list(active):
                    try:
                        next(gg)
                    except StopIteration:
                        active.remove(gg)
                step += 1

        def phase_R():
            with ExitStack() as es:
                pi_ = sb(es, "pos_i", [128, NT], I32)
                pf = sb(es, "pos_f", [128, NT])
                invf = sb(es, "invf", [128, 28])
                ang = sb(es, "ang", [128, NT, 28])
                tq = sb(es, "tq", [128, NT, 28])
                nq = sb(es, "nq", [128, NT, 28])
                rr = sb(es, "rr", [128, NT, 28])
                P.dma("sp", pi_[:], pos_d, w=[pi_])
                P.dma("sp", invf[:], c_invf, w=[invf])
                P.copy("dve", pf[:], pi_[:], r=[pi_], w=[pf])
                P.tt("dve", ang[:], bc(pf[:, :].unsqueeze(2), [128, NT, 28]), bc(invf[:, :].unsqueeze(1), [128, NT, 28]),
                     ALU.mult, r=[pf, invf], w=[ang])
                P.ts("dve", tq[:], ang[:], 1.0 / TWO_PI, None, ALU.mult, None, r=[ang], w=[tq])
                P.ts("dve", nq[:], tq[:], 12582912.0, None, ALU.add, None, r=[tq], w=[nq])
                P.ts("dve", tq[:], nq[:], 12582912.0, None, ALU.subtract, None, r=[nq], w=[tq])
                P.stt("dve", rr[:], tq[:], -CW1, ang[:], ALU.mult, ALU.add, r=[tq, ang], w=[rr])
                P.stt("dve", nq[:], tq[:], -CW2, rr[:], ALU.mult, ALU.add, r=[tq, rr], w=[nq])
                P.stt("dve", rr[:], tq[:], -CW3, nq[:], ALU.mult, ALU.add, r=[tq, nq], w=[rr])
                P.ts("dve", rr[:], rr[:], 3.14159, -3.14159, ALU.min, ALU.max, r=[rr], w=[rr])
                P.act(sinT[:], rr[:], AF.Sin, r=[rr], w=[sinT])
                P.stt("dve", nq[:], rr[:], -1.0, rr[:], ALU.mult, ALU.max, r=[rr], w=[nq])
                P.ts("dve", tq[:], nq[:], -1.0, math.pi / 2, ALU.mult, ALU.add, r=[nq], w=[tq])
                P.act(cosT[:], tq[:], AF.Sin, r=[tq], w=[cosT])
            P.barrier()

        def norm_tile(es_objs, src_d, t, gcol, xnT, pT):
            xt, junk, ss, sd, rstd, xn = es_objs
            P.dma("sp", xt[:], src_d[t * 128:(t + 1) * 128, :], w=[xt])
            P.act(junk[:], xt[:], AF.Square, r=[xt], w=[junk, ss], accum_out=ss[:, 0:1])
            rstd_of(None, ss[:, 0:1], 1024.0, 1, sd, rstd, [ss], None)
            P.ts("dve", xn[:], xt[:], rstd[:, 0:1], None, ALU.mult, None, r=[xt, rstd], w=[xn])
            for c in range(8):
                P.tr(pT[:, c * 128:(c + 1) * 128], xn[:, c * 128:(c + 1) * 128], ident_b[:], r=[xn, ident_b], w=[pT])
            P.tt("dve", xnT[:], pT[:].rearrange("p (c t) -> p c t", c=8), bc(gcol[:, :].unsqueeze(2), [128, 8, 128]),
                 ALU.mult, r=[pT, gcol], w=[xnT])

        def head_rstd(src_f32, nh, dh, sq, ssq, sd, rq, r):
            P.tt("pool", sq[:, 0:nh * dh], src_f32, src_f32, ALU.mult, r=r, w=[sq])
            P.reduce(ssq[:, 0:nh], sq[:, 0:nh * dh].rearrange("p (h d) -> p h d", h=nh), ALU.add, r=[sq], w=[ssq])
            rstd_of(None, ssq[:, 0:nh], float(dh), nh, sd, rq, [ssq], None)

        def phase_A0():
            with ExitStack() as es:
                W = sb(es, "W0", [128, 8, 2560], BF16)
                Wd = [Dep() for _ in range(8)]
                for c in range(8):
                    P.dma("pool", W[:, c, :], w_in0[c * 128:(c + 1) * 128, :], w=[Wd[c]])
                gc = sb(es, "g0c", [128, 8])
                P.dma("sp", gc[:], g0col, w=[gc])
                gq = sb(es, "gq", [128, 1]); gk = sb(es, "gk", [128, 1])
                P.dma("sp", gq[:], gq_d, w=[gq]); P.dma("sp", gk[:], gk_d, w=[gk])
                sgg = sb(es, "sgg", [128, 512])
                P.dma("sp", sgg[:], sgu_g_d.partition_broadcast(128), w=[sgg])
                sgb = sb(es, "sgb", [128, 8])
                P.dma("sp", sgb[:], sgu_b_d, w=[sgb])
                wsf = sb(es, "wsf", [128, 8, 128]); msk = sb(es, "msk", [128, 128])
                WsT = sb(es, "WsT", [128, 8, 128], BF16)
                P.dma("sp", wsf[:], sgu_wT_d, w=[wsf]); P.dma("sp", msk[:], c_sgumask, w=[msk])
                P.tt("dve", WsT[:], wsf[:], bc(msk[:, :].unsqueeze(1), [128, 8, 128]), ALU.mult, r=[wsf, msk], w=[WsT])
                NB = 2
                xt = Ring([sb(es, f"xt{i}", [128, 1024]) for i in range(NB)])
                junk = sb(es, "junk", [128, 1024], BF16)
                ss = Ring([sb(es, f"ss{i}", [128, 1]) for i in range(NB)])
                sd = Ring([sb(es, f"sd{i}", [128, 8]) for i in range(NB)])
                rstd = Ring([sb(es, f"rstd{i}", [128, 1]) for i in range(NB)])
                xn = Ring([sb(es, f"xn{i}", [128, 1024], BF16) for i in range(NB)])
                xnT = Ring([sb(es, f"xnT{i}", [128, 8, 128], BF16) for i in range(NB)])
                def two0(name, shape, dt=F32):
                    return [sb(es, f"{name}_{p}", shape, dt) for p in range(2)]
                qs2 = [two0(f"qs{p}", [128, 512]) for p in range(2)]
                qn2 = [two0(f"qn{p}", [128, 512], BF16) for p in range(2)]
                sq2 = two0("sq", [128, 512]); ssq2 = two0("ssq", [128, 8]); rq2 = two0("rq", [128, 8])
                qTt = Ring([sb(es, f"qTt{i}", [128, 4, 128], BF16) for i in range(4)])
                vb = Ring([sb(es, f"vb{i}", [128, 512], BF16) for i in range(2)])
                xs2 = two0("xs", [128, 1024]); x22 = two0("x2", [128, 1024]); sg2 = two0("sg", [128, 1024])
                gl2 = two0("gl", [128, 1024])
                zn12 = two0("zn1", [128, 512]); znb2 = two0("zn", [128, 512], BF16)
                mb2 = two0("mb", [128, 512]); bo = Ring([sb(es, f"bo{i}", [128, 512], BF16) for i in range(2)])
                pT = ps(es, "pT", [128, 1024], BF16)
                pQ = ps(es, "pQ", [128, 512]); pK = ps(es, "pK", [128, 512]); pV = ps(es, "pV", [128, 512])
                pUZ = ps(es, "pUZ", [128, 1024]); pM = ps(es, "pM", [128, 512])

                def tile_gen0(t):
                    pp_ = t % 2
                    qs = Ring(qs2[pp_]); qn = Ring(qn2[pp_])
                    sq = sq2[pp_]; ssq = ssq2[pp_]; rq = rq2[pp_]
                    xs = xs2[pp_]; x2 = x22[pp_]; sg = sg2[pp_]; gl = gl2[pp_]
                    zn1 = zn12[pp_]; zn = znb2[pp_]; mb = mb2[pp_]
                    o = (xt.get(t), junk, ss.get(t), sd.get(t), rstd.get(t), xn.get(t))
                    xT = xnT.get(t)
                    norm_tile(o, x_d, t, gc, xT, pT)
                    yield
                    for n, pt_ in enumerate((pQ, pK, pV)):
                        for c in range(8):
                            P.mm(pt_[:, :], xT[:, c, :], W[:, c, n * 512:(n + 1) * 512], c == 0, c == 7, r=[xT, Wd[c]], w=[pt_])
                    for n in range(2):
                        for c in range(8):
                            P.mm(pUZ[:, n * 512:(n + 1) * 512], xT[:, c, :], W[:, c, (3 + n) * 512:(4 + n) * 512], c == 0, c == 7,
                                 r=[xT, Wd[c]], w=[pUZ])
                    v_ = vb.get(t)
                    P.copy("act", qs.get(0)[:], pQ[:], r=[pQ], w=[qs.get(0)])
                    P.copy("act", qs.get(1)[:], pK[:], r=[pK], w=[qs.get(1)])
                    P.copy("act", v_[:], pV[:], r=[pV], w=[v_])
                    P.copy("act", xs[:], pUZ[:], r=[pUZ], w=[xs])
                    yield
                    for which, gcol_, dst in ((0, gq, qT0_s), (1, gk, kT0_s)):
                        q_ = qs.get(which); qn_ = qn.get(which); qT_ = qTt.get(2 * t + which)
                        head_rstd(q_[:], 8, 64, sq, ssq, sd.get(t), rq, [q_])
                        P.tt("dve", qn_[:].rearrange("p (h d) -> p h d", h=8), q_[:].rearrange("p (h d) -> p h d", h=8),
                             bc(rq[:, 0:8].unsqueeze(2), [128, 8, 64]), ALU.mult, r=[q_, rq], w=[qn_])
                        yield
                        for i in range(4):
                            P.tr(pT[:, i * 128:(i + 1) * 128], qn_[:, i * 128:(i + 1) * 128], ident_b[:], r=[qn_, ident_b], w=[pT])
                        P.ts("dve", qT_[:], pT[:, 0:512].rearrange("p (i t) -> p i t", i=4), gcol_[:, 0:1],
                             0.125 if which == 0 else 1.0, ALU.mult, ALU.mult, r=[pT, gcol_], w=[qT_])
                        P.dma("sp", dst[:, :, t * 128:(t + 1) * 128].rearrange("i p t -> p i t"), qT_[:], r=[qT_])
                        yield
                    P.dma("sp", v0_s[t * 128:(t + 1) * 128, :], v_[:], r=[v_])
                    P.tt("pool", x2[:], xs[:], xs[:], ALU.mult, r=[xs], w=[x2])
                    P.ts("pool", x2[:], x2[:], 0.044715, 1.0, ALU.mult, ALU.add, r=[x2], w=[x2])
                    P.tt("pool", x2[:], x2[:], xs[:], ALU.mult, r=[x2, xs], w=[x2])
                    yield
                    P.act(sg[:], x2[:], AF.Sigmoid, r=[x2], w=[sg], scale=1.5957691216057308)
                    P.tt("pool", gl[:], sg[:], xs[:], ALU.mult, r=[sg, xs], w=[gl])
                    yield
                    head_rstd(gl[:, 512:1024], 8, 64, sq, ssq, sd.get(t), rq, [gl])
                    P.tt("dve", zn1[:].rearrange("p (h d) -> p h d", h=8), gl[:, 512:1024].rearrange("p (h d) -> p h d", h=8),
                         bc(rq[:, 0:8].unsqueeze(2), [128, 8, 64]), ALU.mult, r=[gl, rq], w=[zn1])
                    P.tt("pool", zn[:], zn1[:], sgg[:], ALU.mult, r=[zn1, sgg], w=[zn])
                    yield
                    for g in range(8):
                        P.mm(pM[:, g * 64:(g + 1) * 64], WsT[:, g, :], zn[:, g * 64:(g + 1) * 64], True, True, r=[WsT, zn], w=[pM])
                    P.tt("dve", mb[:].rearrange("p (h d) -> p h d", h=8), pM[:].rearrange("p (h d) -> p h d", h=8),
                         bc(sgb[:, 0:8].unsqueeze(2), [128, 8, 64]), ALU.add, r=[pM, sgb], w=[mb])
                    bo_ = bo.get(t)
                    P.tt("pool", bo_[:], mb[:], gl[:, 0:512], ALU.mult, r=[mb, gl], w=[bo_])
                    P.dma("sp", mixed_s[t * 128:(t + 1) * 128, 512:1024], bo_[:], r=[bo_])

                run_rolling(tile_gen0, NT, 5)
            P.barrier()

        def attention(kind, qT_d, kT_d, v_d, col0, scale):
            mla = kind == "mla"
            DP = 96 if mla else 128
            NP = 8 if mla else 4
            D = 96 if mla else 64
            with ExitStack() as es:
                kT = sb(es, "kT", [DP, NP, S], BF16)
                kTd = [Dep() for _ in range(NP)]
                for i in range(NP):
                    P.dma("sp", kT[:, i, :], kT_d[i], w=[kTd[i]])
                vS = sb(es, "vS", [128, NT, 8, 65], BF16)
                P.memset("pool", vS[:], 1.0, w=[vS])
                for t0 in range(NT):
                    P.dma("sp", vS[:, t0, :, 0:64], v_d[t0 * 128:(t0 + 1) * 128, :].rearrange("p (h d) -> p h d", h=8), w=[vS])
                negm = sb(es, "negm", [128, 4, 512], BF16)
                if kind != "dsa":
                    src = c_negsb if kind == "sb" else c_negcc
                    P.dma("pool", negm[:], src.rearrange("j p q -> p j q"), w=[negm])
                qTg = Ring([sb(es, f"qTg{i}", [DP, NP, 512], BF16) for i in range(2)])
                oS = Ring([sb(es, f"oS{i}", [128, 4, 512], BF16) for i in range(2)])
                pt = Ring([sb(es, f"pt{i}", [128, 512], BF16) for i in range(5)])
                rec = Ring([sb(es, f"rec{i}", [128, 4]) for i in range(2)])
                psc = Ring([ps(es, f"psc{i}", [128, 512]) for i in range(3)])
                pacc = Ring([ps(es, f"pacc{i}", [128, 512]) for i in range(2)])
                if kind == "sb":
                    tri = sb(es, "tri", [128, 128], BF16); onesn = sb(es, "onesn", [128, 128], BF16)
                    P.dma("pool", tri[:], c_tri, w=[tri])
                    P.memset("pool", onesn[:], -1.0, w=[onesn])
                    eS = Ring([sb(es, f"eS{i}", [128, 512]) for i in range(2)])
                    spS = Ring([sb(es, f"spS{i}", [128, 512]) for i in range(2)])
                    spB = Ring([sb(es, f"spB{i}", [128, 512], BF16) for i in range(2)])
                    t1S = Ring([sb(es, f"t1S{i}", [128, 512]) for i in range(2)])
                    lacc = sb(es, "lacc", [128, 512]); laccB = Ring([sb(es, f"laccB{i}", [128, 512], BF16) for i in range(2)])
                    plat = Ring([ps(es, f"plat{i}", [128, 512]) for i in range(2)])
                if kind == "dsa":
                    ikT = sb(es, "ikT", [64, S], BF16)
                    P.dma("sp", ikT[:], ikT_s, w=[ikT])
                    aTg = Ring([sb(es, f"aTg{i}", [64, 4, 512], BF16) for i in range(2)])
                    score = sb(es, "score", [128, S])
                    NM = sb(es, "NM", [128, S], BF16)
                    NMT = sb(es, "NMT", [128, NT, 512], BF16)
                    Rb = Ring([sb(es, f"Rb{i}", [128, 512]) for i in range(4)])
                    accA = sb(es, "accA", [128, 512]); accB = sb(es, "accB", [128, 512])
                    admb = sb(es, "admb", [128, 128]); pw2 = sb(es, "pw2", [128, NBIS + 1])
                    P.dma("sp", admb[:], c_adm, w=[admb]); P.dma("sp", pw2[:], c_pow2, w=[pw2])
                    dtab = sb(es, "dtab", [128, NBIS + 1]); dtab2 = sb(es, "dtab2", [128, NBIS + 1])
                    mn = sb(es, "mn", [128, 1]); mx = sb(es, "mx", [128, 1]); d0 = sb(es, "d0", [128, 1])
                    mid = sb(es, "mid", [128, 1]); cnt = sb(es, "cnt", [128, 1]); gp = sb(es, "gp", [128, 1])
                    tau = sb(es, "tau", [128, 1]); cjunk = sb(es, "cjunk", [128, S], BF16)
                    plog = Ring([ps(es, f"plog{i}", [128, 512]) for i in range(2)])
                    pTm = ps(es, "pTm", [128, 1024], BF16)
                    pTmd = [Dep(), Dep()]
                    NMTd = [Dep() for _ in range(NT)]
                    stg = Ring([sb(es, f"stg{i}", [128, 4, 128], BF16) for i in range(4)])
                    P.memset("pool", NMT[:], NEG, w=NMTd)
                    nmt_w = {}
                    grpc = [0]

                    def prep_thunks(G):
                        th = []
                        a_ = aTg.get(G)
                        th.append(lambda: P.dma("sp", a_[:], aT_s[:, :, G * 512:(G + 1) * 512].rearrange("i p t -> p i t"), w=[a_]))
                        for j in range(4):
                            qb = 4 * G + j
                            nvis = (qb + 1) * 128
                            sc_ = score[:, 0:nvis]
                            for kc in range(0, nvis, 512):
                                wdt = min(512, nvis - kc)
                                for h in range(8):
                                    def f_idx(h=h, kc=kc, wdt=wdt, qb=qb, j=j):
                                        pl_ = plog.get(h)
                                        b0 = (h % 2) * 32
                                        P.mm(pl_[:, 0:wdt], a_[b0:b0 + 32, h // 2, j * 128:(j + 1) * 128], ikT[b0:b0 + 32, kc:kc + wdt],
                                             True, True, r=[a_, ikT], w=[pl_])
                                        R_ = Rb.get(h)
                                        P.act(R_[:, 0:wdt], pl_[:, 0:wdt], AF.Relu, r=[pl_], w=[R_])
                                        acc = accA if h < 4 else accB
                                        sg_ = sgnT[:, qb, h:h + 1]
                                        if h % 4 == 0:
                                            P.ts("dve", acc[:, 0:wdt], R_[:, 0:wdt], sg_, None, ALU.mult, None, r=[R_, sgnT], w=[acc])
                                        else:
                                            P.stt("dve", acc[:, 0:wdt], R_[:, 0:wdt], sg_, acc[:, 0:wdt], ALU.mult, ALU.add,
                                                  r=[R_, sgnT, acc], w=[acc])
                                    th.append(f_idx)
                                th.append(lambda kc=kc, wdt=wdt: P.tt("dve", score[:, kc:kc + wdt], accA[:, 0:wdt], accB[:, 0:wdt], ALU.add,
                                                                      r=[accA, accB], w=[score]))

                            def f_init(qb=qb, nvis=nvis, sc_=sc_):
                                P.reduce(mn[:], sc_, ALU.min, r=[score], w=[mn])
                                P.reduce(mx[:], sc_, ALU.max, r=[score], w=[mx])
                                P.tt("dve", score[:, qb * 128:nvis], score[:, qb * 128:nvis], admb[:], ALU.add, r=[score, admb], w=[score])
                                P.tt("dve", d0[:], mx[:], mn[:], ALU.subtract, r=[mx, mn], w=[d0])
                                P.ts("dve", d0[:], d0[:], 1.001, 1e-6, ALU.mult, ALU.add, r=[d0], w=[d0])
                                P.ts("dve", dtab[:], pw2[:], d0[:, 0:1], None, ALU.mult, None, r=[pw2, d0], w=[dtab])
                                P.ts("dve", dtab2[:], dtab[:], 2.0, None, ALU.mult, None, r=[dtab], w=[dtab2])
                                P.tt("dve", mid[:], mn[:], dtab[:, 0:1], ALU.add, r=[mn, dtab], w=[mid])
                            th.append(f_init)
                            for k in range(NBIS):
                                def f_bis(k=k, nvis=nvis, sc_=sc_):
                                    P.ts("dve", cjunk[:, 0:nvis], sc_, mid[:, 0:1], None, ALU.is_ge, ALU.add, r=[score, mid], w=[cjunk, cnt],
                                         accum_out=cnt[:, 0:1])
                                    P.stt("dve", gp[:], cnt[:], float(TOPK) - 0.5, dtab2[:, k + 1:k + 2], ALU.is_ge, ALU.mult,
                                          r=[cnt, dtab2], w=[gp])
                                    P.stt("dve", mid[:], mid[:], dtab[:, k + 1:k + 2], gp[:], ALU.subtract, ALU.add, r=[mid, dtab, gp], w=[mid])
                                th.append(f_bis)

                            def f_fin(nvis=nvis, sc_=sc_):
                                P.tt("dve", tau[:], mid[:], dtab2[:, NBIS:NBIS + 1], ALU.subtract, r=[mid, dtab2], w=[tau])
                                P.ts("dve", NM[:, 0:nvis], sc_, tau[:, 0:1], NEG, ALU.is_lt, ALU.mult, r=[score, tau], w=[NM])
                            th.append(f_fin)
                            for k0 in range(0, qb + 1, 4):
                                def f_tr(k0=k0, qb=qb, j=j):
                                    k1 = min(k0 + 3, qb)
                                    n_ = k1 - k0 + 1
                                    par = 0
                                    stg_ = stg.get(grpc[0])
                                    grpc[0] += 1
                                    for kt in range(k0, k1 + 1):
                                        c0 = par * 512 + (kt - k0) * 128
                                        P.tr(pTm[:, c0:c0 + 128], NM[:, kt * 128:(kt + 1) * 128], ident_b[:],
                                             r=[NM, ident_b], w=[pTm])
                                    P.copy("act", stg_[:, 0:n_, :],
                                           pTm[:, par * 512:par * 512 + n_ * 128].rearrange("p (k q) -> p k q", k=n_), r=[pTm], w=[stg_])
                                    dd = Dep()
                                    nmt_w[(G, j, k0 // 4)] = dd
                                    P.dma("sp", NMT_d[G, k0:k1 + 1, :, j * 128:(j + 1) * 128].rearrange("k p q -> p k q"), stg_[:, 0:n_, :],
                                          r=[stg_], w=[dd])
                                th.append(f_tr)
                        return th
                ui = 0
                for G in range(NG):
                    q_ = qTg.get(G)
                    P.dma("sp", q_[:], qT_d[:, :, G * 512:(G + 1) * 512].rearrange("i p t -> p i t"), w=[q_])
                    nkt = 4 * G + 4
                    nxt = []
                    if kind == "dsa":
                        if G == 0:
                            for f in prep_thunks(0):
                                f()
                        if G + 1 < NG:
                            nxt = prep_thunks(G + 1)
                            if NOINT:
                                for f in nxt:
                                    f()
                                nxt = []
                        for kt in range(nkt):
                            j0 = max(0, kt - 4 * G)
                            P.dma("sp", NMT[:, kt, j0 * 128:512], NMT_d[G, kt, :, j0 * 128:512],
                                  r=[nmt_w[(G, j, kt // 4)] for j in range(j0, 4)], w=[NMTd[kt]])
                    o_ = oS.get(G)
                    units = [(h, kt) for h in range(8) for kt in range(nkt)]
                    ui0 = ui
                    pts = {}

                    def emit_QK(u):
                        h, kt = units[u]
                        pr = h if mla else h // 2
                        b0 = 0 if mla else (h % 2) * 64
                        sc = psc.get(ui0 + u)
                        diag = kt >= 4 * G
                        masked = diag or kind == "dsa"
                        P.mm(sc[:], kT[b0:b0 + D, pr, kt * 128:(kt + 1) * 128], q_[b0:b0 + D, pr, :], True, not masked,
                             r=[kTd[pr], q_], w=[sc])
                        if masked:
                            if kind == "dsa":
                                P.mm(sc[:], ident_b[:], NMT[:, kt, :], False, True, r=[ident_b, NMTd[kt]], w=[sc])
                            else:
                                P.mm(sc[:], ident_b[:], negm[:, kt - 4 * G, :], False, True, r=[ident_b, negm], w=[sc])
                        p_ = pt.get(ui0 + u)
                        P.act(p_[:], sc[:], AF.Exp, r=[sc], w=[p_], scale=scale)
                        pts[u] = p_

                    def emit_PV(u):
                        h, kt = units[u]
                        accT = pacc.get(G * 8 + h)
                        acc = accT[:, 0:260].rearrange("p (j c) -> p j c", j=4)
                        p_ = pts.pop(u)
                        js = [j for j in range(4) if kt <= 4 * G + j]
                        for j in js:
                            P.mm(acc[:, j, :], p_[:, j * 128:(j + 1) * 128], vS[:, kt, h, :], kt == 0 and j == js[0],
                                 kt == nkt - 1 and j == js[-1], r=[p_, vS], w=[accT])
                        if kt == nkt - 1:
                            rec_ = rec.get(G * 8 + h)
                            P.recip(rec_[:], acc[:, :, 64], r=[accT], w=[rec_])
                            P.tt("dve", o_[:, :, h * 64:(h + 1) * 64], acc[:, :, 0:64], bc(rec_[:, 0:4].unsqueeze(2), [128, 4, 64]),
                                 ALU.mult, r=[accT, rec_], w=[o_])

                    emit_QK(0)
                    emit_QK(1)
                    ti = 0
                    for u in range(len(units)):
                        if u + 2 < len(units):
                            emit_QK(u + 2)
                        emit_PV(u)
                        tgt = (u + 1) * len(nxt) // len(units)
                        while ti < tgt:
                            nxt[ti]()
                            ti += 1
                    ui += len(units)
                    P.dma("sp", mixed_s[G * 512:(G + 1) * 512, col0:col0 + 512].rearrange("(j p) c -> p j c", p=128), o_[:], r=[o_])
            P.barrier()

        def attention_sb(qT_d, kT_d, v_d, col0):
            with ExitStack() as es:
                kT = sb(es, "kT", [128, 4, S], BF16)
                kTd = [Dep() for _ in range(4)]
                for i in range(4):
                    P.dma("sp", kT[:, i, :], kT_d[i], w=[kTd[i]])
                vS = sb(es, "vS", [128, NT, 8, 64], BF16)
                for t0 in range(NT):
                    P.dma("sp", vS[:, t0, :, :], v_d[t0 * 128:(t0 + 1) * 128, :].rearrange("p (h d) -> p h d", h=8), w=[vS])
                negm = sb(es, "negm", [128, 4, 512], BF16)
                P.dma("pool", negm[:], c_negsb.rearrange("j p q -> p j q"), w=[negm])
                trii = sb(es, "trii", [128, 128], BF16); onesn = sb(es, "onesn", [128, 128], BF16)
                P.dma("pool", trii[:], c_trii, w=[trii])
                P.memset("dve", onesn[:], -1.0, w=[onesn])
                qTg = Ring([sb(es, f"qTg{i}", [128, 4, 512], BF16) for i in range(2)])
                oS = Ring([sb(es, f"oS{i}", [128, 4, 512], BF16) for i in range(2)])
                eS = Ring([sb(es, f"eS{i}", [128, 512]) for i in range(4)])
                spB = Ring([sb(es, f"spB{i}", [128, 512], BF16) for i in range(8)])
                pt = Ring([sb(es, f"pt{i}", [128, 512], BF16) for i in range(6)])
                lacc = [sb(es, f"lacc{i}", [128, 512]) for i in range(2)]
                laccB = [Ring([sb(es, f"laccB{i}_{k}", [128, 512], BF16) for k in range(4)]) for i in range(2)]
                psZ = Ring([ps(es, f"psZ{i}", [128, 512]) for i in range(3)])
                psL = Ring([ps(es, f"psL{i}", [128, 512]) for i in range(3)])
                pacc = [ps(es, f"pacc{i}", [128, 512]) for i in range(2)]
                steps = []
                for G in range(NG):
                    nkt = 4 * G + 4
                    for hp in range(4):
                        for i in range(nkt):
                            steps.append((G, hp, i, nkt))
                st = {}
                qs_ = {}
                uzc = [0]

                def stage1(s):
                    G, hp, i, nkt = steps[s]
                    for Gl in ([0] if s == 0 else []) + ([G + 1] if (hp == 1 and i == 0 and G + 1 < NG) else []):
                        ql = qTg.get(Gl)
                        P.dma("sp", ql[:], qT_d[:, :, Gl * 512:(Gl + 1) * 512].rearrange("i p t -> p i t"), w=[ql])
                        qs_[Gl] = ql
                    q_ = qs_[G]
                    for hs in range(2):
                        b0 = hs * 64
                        kt = nkt - 1 - i
                        diag = kt >= 4 * G
                        uz = 2 * s + hs
                        Z = psZ.get(uz); e_ = eS.get(uz); sp_ = spB.get(uz)
                        P.mm(Z[:], kT[b0:b0 + 64, hp, kt * 128:(kt + 1) * 128], q_[b0:b0 + 64, hp, :], True, not diag,
                             r=[kTd[hp], q_], w=[Z])
                        if diag:
                            P.mm(Z[:], ident_b[:], negm[:, kt - 4 * G, :], False, True, r=[ident_b, negm], w=[Z])
                        P.act(e_[:], Z[:], AF.Exp, r=[Z], w=[e_])
                        P.act(sp_[:], e_[:], AF.Ln, r=[e_], w=[sp_], bias=1.0)
                        lb = laccB[hs].get(s)
                        if i == 0:
                            P.copy("dve", lb[:], sp_[:], r=[sp_], w=[lb])
                            P.copy("dve", lacc[hs][:], sp_[:], r=[sp_], w=[lacc[hs]])
                        elif i < nkt - 1:
                            P.tt("dve", lb[:], lacc[hs][:], sp_[:], ALU.add, r=[lacc[hs], sp_], w=[lb])
                            P.tt("dve", lacc[hs][:], lacc[hs][:], sp_[:], ALU.add, r=[lacc[hs], sp_], w=[lacc[hs]])
                        st[(hs, s)] = [sp_, lb, kt, diag, None]

                def stage2(s):
                    G, hp, u, nkt = steps[s]
                    q_ = qs_[G]
                    for hs in range(2):
                        b0 = hs * 64
                        sp_, _, kt, diag, _ = st[(hs, s)]
                        uz = 2 * s + hs
                        Lp = psL.get(uz); p_ = pt.get(uz)
                        mms = [(kT[b0:b0 + 64, hp, kt * 128:(kt + 1) * 128], q_[b0:b0 + 64, hp, :], [kTd[hp], q_]),
                               (trii[:], sp_[:], [trii, sp_])]
                        if u > 0:
                            lbp = st[(hs, s - 1)][1]
                            mms.append((onesn[:], lbp[:], [onesn, lbp]))
                        if diag:
                            mms.append((ident_b[:], negm[:, kt - 4 * G, :], [ident_b, negm]))
                        for mi, (l_, r_, rd) in enumerate(mms):
                            P.mm(Lp[:], l_, r_, mi == 0, mi == len(mms) - 1, r=rd, w=[Lp])
                        P.act(p_[:], Lp[:], AF.Exp, r=[Lp], w=[p_])
                        st[(hs, s)][4] = p_

                def stage3(s):
                    G, hp, u, nkt = steps[s]
                    o_ = oS.get(G)
                    for hs in range(2):
                        h = 2 * hp + hs
                        _, _, kt, diag, p_ = st[(hs, s)]
                        accT = pacc[hs]
                        acc = accT[:, 0:256].rearrange("p (j c) -> p j c", j=4)
                        js = [j for j in range(4) if kt <= 4 * G + j]
                        for j in js:
                            P.mm(acc[:, j, :], p_[:, j * 128:(j + 1) * 128], vS[:, kt, h, :], u == 0 and j == js[0],
                                 u == nkt - 1 and j == js[-1], r=[p_, vS], w=[accT])
                        if u == nkt - 1:
                            P.copy("act", o_[:, :, h * 64:(h + 1) * 64], accT[:, 0:256].rearrange("p (j c) -> p j c", j=4),
                                   r=[accT], w=[o_])
                    if s >= 2:
                        st.pop((0, s - 2), None); st.pop((1, s - 2), None)
                    if u == nkt - 1 and hp == 3:
                        P.dma("sp", mixed_s[G * 512:(G + 1) * 512, col0:col0 + 512].rearrange("(j p) c -> p j c", p=128), o_[:], r=[o_])

                NS = len(steps)
                for s in range(NS + 2):
                    if s < NS:
                        stage1(s)
                    if 0 <= s - 1 < NS:
                        stage2(s - 1)
                    if 0 <= s - 2 < NS:
                        stage3(s - 2)
            P.barrier()

        def phase_CD(layer, hin_d, hout_d, w_out_d):
            with ExitStack() as es:
                Wo = sb(es, "Wo", [128, 8, 1024], BF16)
                Wod = [Dep() for _ in range(8)]
                for c in range(8):
                    P.dma("pool", Wo[:, c, :], w_out_d[c * 128:(c + 1) * 128, :], w=[Wod[c]])
                g2 = sb(es, "g2", [128, 8])
                P.dma("sp", g2[:], g2col[layer], w=[g2])
                Wr = sb(es, "Wr", [128, 8, 20])
                P.dma("sp", Wr[:], wr_d[layer].rearrange("(c p) n -> p c n", p=128), w=[Wr])
                rb = sb(es, "rb", [128, 20])
                P.dma("sp", rb[:], rb_d[layer].partition_broadcast(128), w=[rb])
                hS = [sb(es, f"hS{j}", [128, 1024]) for j in range(SBT)]
                hP = Ring([sb(es, f"hP{i}", [128, 1024]) for i in range(2)])
                hm_dep = [Dep() for _ in range(NT)]
                xTb = [sb(es, f"xTb{i}", [128, 8, SBT * 128], BF16) for i in range(2)]
                xTbd = [[Dep() for _ in range(SBT)] for _ in range(2)]
                gates = [[sb(es, f"gates{i}_{j}", [128, 16]) for j in range(SBT)] for i in range(2)]
                mx_ = Ring([sb(es, f"mxd{i}", [128, 1024], BF16) for i in range(2)])
                mT = Ring([sb(es, f"mT{i}", [128, 8, 128], BF16) for i in range(2)])
                junk = sb(es, "junkc", [128, 1024], BF16)
                ss = sb(es, "ssc", [128, 1]); sd = sb(es, "sdc", [128, 8]); rstd = sb(es, "rstdc", [128, 1])
                xn = sb(es, "xnc", [128, 1024]); xTf = sb(es, "xTf", [128, 8, 128])
                rl = sb(es, "rl", [128, 20]); m4 = sb(es, "m4", [128, 1]); e4 = sb(es, "e4", [128, 4]); s4 = sb(es, "s4", [128, 1])
                pg_ = sb(es, "pg_", [128, 1]); oh = sb(es, "oh", [128, 4]); tm = sb(es, "tm", [128, 16]); il = sb(es, "il", [128, 4])
                ex = sb(es, "ex", [128, 4]); o1 = sb(es, "o1", [128, 4]); ex2 = sb(es, "ex2", [128, 4]); m2 = sb(es, "m2", [128, 1])
                o2 = sb(es, "o2", [128, 4]); den = sb(es, "den", [128, 1]); gi = sb(es, "gi", [128, 4])
                wg = Ring([sb(es, f"wg{i}", [128, 8, 512], BF16) for i in range(2)])
                wu = Ring([sb(es, f"wu{i}", [128, 8, 512], BF16) for i in range(2)])
                wd = Ring([sb(es, f"wd{i}", [128, 4, 1024], BF16) for i in range(2)])
                sgs = Ring([sb(es, f"sgs{i}", [128, 512]) for i in range(4)])
                hid = Ring([sb(es, f"hid{i}", [128, 512], BF16) for i in range(4)])
                hT = Ring([sb(es, f"hT{i}", [128, 4, 128], BF16) for i in range(4)])
                pT = ps(es, "pTc", [128, 1024], BF16)
                pO = ps(es, "pO", [128, 1024])
                pGs = [ps(es, f"pG{i}", [128, 512]) for i in range(2)]
                pUs = [ps(es, f"pU{i}", [128, 512]) for i in range(2)]
                pX = pO
                pR = ps(es, "pR", [128, 512])

                def prep_thunks(sbi):
                    par = sbi % 2
                    th = []

                    def mk(sbi, j):
                        t = sbi * SBT + j
                        h_ = hP.get(t); m_ = mx_.get(t); mT_ = mT.get(t)

                        def T0():
                            P.dma("sp", m_[:], mixed_s[t * 128:(t + 1) * 128, :], w=[m_])
                            P.dma("sp", h_[:], hin_d[t * 128:(t + 1) * 128, :], w=[h_])

                        def T1():
                            for c in range(8):
                                P.tr(pT[:, c * 128:(c + 1) * 128], m_[:, c * 128:(c + 1) * 128], ident_b[:], r=[m_, ident_b], w=[pT])
                            P.copy("act", mT_[:], pT[:].rearrange("p (c t) -> p c t", c=8), r=[pT], w=[mT_])

                        def T2():
                            for n in range(2):
                                for c in range(8):
                                    P.mm(pO[:, n * 512:(n + 1) * 512], mT_[:, c, :], Wo[:, c, n * 512:(n + 1) * 512], c == 0, c == 7,
                                         r=[mT_, Wod[c]], w=[pO])
                            P.tt("dve", h_[:], pO[:], h_[:], ALU.add, r=[pO, h_], w=[h_])
                            P.dma("sp", hmid_s[t * 128:(t + 1) * 128, :], h_[:], r=[h_], w=[hm_dep[t]])

                        def T3():
                            P.act(junk[:], h_[:], AF.Square, r=[h_], w=[junk, ss], accum_out=ss[:, 0:1])
                            rstd_of(None, ss[:, 0:1], 1024.0, 1, sd, rstd, [ss], None)
                            P.ts("dve", xn[:], h_[:], rstd[:, 0:1], None, ALU.mult, None, r=[h_, rstd], w=[xn])

                        def T4():
                            for c in range(8):
                                P.tr(pX[:, c * 128:(c + 1) * 128], xn[:, c * 128:(c + 1) * 128], ident_f[:], r=[xn, ident_f], w=[pX])
                            P.tt("dve", xTf[:], pX[:].rearrange("p (c t) -> p c t", c=8), bc(g2[:, :].unsqueeze(2), [128, 8, 128]),
                                 ALU.mult, r=[pX, g2], w=[xTf])
                            P.copy("pool", xTb[par][:, :, j * 128:(j + 1) * 128], xTf[:], r=[xTf], w=[xTbd[par][j]])

                        def T5():
                            for c in range(8):
                                P.mm(pR[:, 0:20], xTf[:, c, :], Wr[:, c, :], c == 0, c == 7, r=[xTf, Wr], w=[pR])
                            P.tt("dve", rl[:], pR[:, 0:20], rb[:], ALU.add, r=[pR, rb], w=[rl])
                            P.reduce(m4[:], rl[:, 0:4], ALU.max, r=[rl], w=[m4])
                            P.ts("dve", oh[:], rl[:, 0:4], m4[:, 0:1], None, ALU.is_ge, None, r=[rl, m4], w=[oh])
                            P.ts("dve", e4[:], rl[:, 0:4], m4[:, 0:1], None, ALU.subtract, None, r=[rl, m4], w=[e4])
                            P.act(e4[:], e4[:], AF.Exp, r=[e4], w=[e4])
                            P.reduce(s4[:], e4[:], ALU.add, r=[e4], w=[s4])
                            P.recip(pg_[:], s4[:], r=[s4], w=[pg_])
                            P.tt("dve", tm[:].rearrange("p (g e) -> p g e", g=4), rl[:, 4:20].rearrange("p (g e) -> p g e", g=4),
                                 bc(oh[:, 0:4].unsqueeze(2), [128, 4, 4]), ALU.mult, r=[rl, oh], w=[tm])
                            P.reduce(il[:], tm[:].rearrange("p (g e) -> p e g", g=4), ALU.add, r=[tm], w=[il])
                            P.reduce(m4[:], il[:], ALU.max, r=[il], w=[m4])
                            P.ts("dve", ex[:], il[:], m4[:, 0:1], None, ALU.subtract, None, r=[il, m4], w=[ex])
                            P.act(ex[:], ex[:], AF.Exp, r=[ex], w=[ex])
                            P.ts("dve", o1[:], il[:], m4[:, 0:1], None, ALU.is_ge, None, r=[il, m4], w=[o1])
                            P.stt("dve", ex2[:], o1[:], -2.0, ex[:], ALU.mult, ALU.add, r=[o1, ex], w=[ex2])
                            P.reduce(m2[:], ex2[:], ALU.max, r=[ex2], w=[m2])
                            P.ts("dve", o2[:], ex2[:], m2[:, 0:1], None, ALU.is_ge, None, r=[ex2, m2], w=[o2])
                            P.ts("dve", den[:], m2[:], 1.0, None, ALU.add, None, r=[m2], w=[den])
                            P.recip(den[:], den[:], r=[den], w=[den])
                            P.tt("dve", den[:], den[:], pg_[:], ALU.mult, r=[den, pg_], w=[den])
                            P.tt("dve", o1[:], o1[:], o2[:], ALU.add, r=[o1, o2], w=[o1])
                            P.tt("dve", gi[:], o1[:], ex[:], ALU.mult, r=[o1, ex], w=[gi])
                            P.ts("dve", gi[:], gi[:], den[:, 0:1], None, ALU.mult, None, r=[gi, den], w=[gi])
                            P.tt("dve", gates[par][j][:].rearrange("p (g e) -> p g e", g=4), bc(oh[:, 0:4].unsqueeze(2), [128, 4, 4]),
                                 bc(gi[:, 0:4].unsqueeze(1), [128, 4, 4]), ALU.mult, r=[oh, gi], w=[gates[par][j]])
                        return T0, [T1, T2, T3, T4, T5]

                    parts = [mk(sbi, j) for j in range(SBT)]
                    th.append(parts[0][0])
                    for j in range(SBT):
                        if j + 1 < SBT:
                            th.append(parts[j + 1][0])
                        th.extend(parts[j][1])
                    return th

                for f in prep_thunks(0):
                    f()
                for sbi in range(NSB):
                    par = sbi % 2
                    nxt = prep_thunks(sbi + 1) if sbi + 1 < NSB else []
                    for j in range(SBT):
                        t = sbi * SBT + j
                        P.dma("sp", hS[j][:], hmid_s[t * 128:(t + 1) * 128, :], r=[hm_dep[t]], w=[hS[j]])
                    units = [(e, j) for e in range(16) for j in range(SBT)]
                    ubase = sbi * len(units)

                    def emit_GU(k):
                        e, j = units[k]
                        it = sbi * 16 + e
                        wg_ = wg.get(it); wu_ = wu.get(it); wd_ = wd.get(it)
                        if j == 0:
                            P.dma("pool", wg_[:], wg_d[layer][e].rearrange("(c p) n -> p c n", p=128), w=[wg_])
                            P.dma("pool", wu_[:], wu_d[layer][e].rearrange("(c p) n -> p c n", p=128), w=[wu_])
                            P.dma("pool", wd_[:], wd_d[layer][e].rearrange("(c p) n -> p c n", p=128), w=[wd_])
                        pG = pGs[k % 2]; pU = pUs[k % 2]
                        for c in range(8):
                            P.mm(pG[:], xTb[par][:, c, j * 128:(j + 1) * 128], wg_[:, c, :], c == 0, c == 7, r=[xTbd[par][j], wg_], w=[pG])
                        for c in range(8):
                            P.mm(pU[:], xTb[par][:, c, j * 128:(j + 1) * 128], wu_[:, c, :], c == 0, c == 7, r=[xTbd[par][j], wu_], w=[pU])
                        sg_ = sgs.get(ubase + k); hd_ = hid.get(ubase + k)
                        P.act(sg_[:], pG[:], AF.Silu, r=[pG], w=[sg_])
                        P.stt("dve", hd_[:], sg_[:], gates[par][j][:, e:e + 1], pU[:], ALU.mult, ALU.mult, r=[sg_, gates[par][j], pU], w=[hd_])

                    def emit_T(k):
                        hd_ = hid.get(ubase + k); hT_ = hT.get(ubase + k)
                        for c in range(4):
                            P.tr(pT[:, c * 128:(c + 1) * 128], hd_[:, c * 128:(c + 1) * 128], ident_b[:], r=[hd_, ident_b], w=[pT])
                        P.copy("act", hT_[:], pT[:, 0:512].rearrange("p (c t) -> p c t", c=4), r=[pT], w=[hT_])

                    def emit_D(k):
                        e, j = units[k]
                        it = sbi * 16 + e
                        wd_ = wd.get(it)
                        hT_ = hT.get(ubase + k)
                        for n in range(2):
                            for c in range(4):
                                P.mm(pO[:, n * 512:(n + 1) * 512], hT_[:, c, :], wd_[:, c, n * 512:(n + 1) * 512], c == 0, c == 3,
                                     r=[hT_, wd_], w=[pO])
                        P.tt("dve", hS[j][:], pO[:], hS[j][:], ALU.add, r=[pO, hS[j]], w=[hS[j]])

                    emit_GU(0)
                    emit_GU(1)
                    emit_T(0)
                    ti = 0
                    for k in range(len(units)):
                        if k + 2 < len(units):
                            emit_GU(k + 2)
                        if k + 1 < len(units):
                            emit_T(k + 1)
                        emit_D(k)
                        tgt = (k + 1) * len(nxt) // len(units)
                        while ti < tgt:
                            nxt[ti]()
                            ti += 1
                    for j in range(SBT):
                        t = sbi * SBT + j
                        P.dma("sp", hout_d[t * 128:(t + 1) * 128, :], hS[j][:], r=[hS[j]])
            P.barrier()

        phase_R()
        phase_A0()
        if stop != "a0":
            attention_sb(qT0_s, kT0_s, v0_s, 0)
        if stop in ("a0", "sb"):
            pass
        elif stop in ("mix0", "l0"):
            phase_CD(0, x_d, out_d, w_out0)
            P.barrier()
        else:
            phase_CD(0, x_d, h1_s, w_out0)
            phase_A1_holder[0](locals())
        P.emit()
    return nc


phase_A1_holder = [None]


def _consts(NT):
    c = {}
    c["c_ident"] = np.eye(128, dtype=np.float32)
    j = np.arange(128)
    c["c_sgumask"] = ((j[:, None] // 64) <= (j[None, :] // 64)).astype(np.float32)
    negsb = np.zeros((4, 128, 512), np.float32)
    negcc = np.zeros((4, 128, 512), np.float32)
    q = np.arange(512)
    for jj in range(4):
        s = jj * 128 + np.arange(128)
        negsb[jj] = np.where(s[:, None] < q[None, :], 0.0, NEG)
        negcc[jj] = np.where((s[:, None] // 64) <= (q[None, :] // 64), 0.0, NEG)
    c["c_negsb"] = negsb
    c["c_negcc"] = negcc
    c["c_adm"] = np.where((j[None, :] // 64) <= (j[:, None] // 64), 0.0, -1e30).astype(np.float32)
    fr = []
    for rot in (32, 16, 8):
        half = rot // 2
        fr.append(np.power(np.float32(500000.0), -np.arange(half, dtype=np.float32) * np.float32(2.0 / rot)).astype(np.float32))
    c["c_invf"] = np.tile(np.concatenate(fr)[None, :], (128, 1)).astype(np.float32)
    c["c_pow2"] = np.tile((2.0 ** -(np.arange(NBIS + 1) + 1.0))[None, :], (128, 1)).astype(np.float32)
    c["c_tri"] = np.where(j[:, None] > j[None, :], -1.0, 0.0).astype(np.float32)
    c["c_trii"] = np.where(j[:, None] >= j[None, :], -1.0, 0.0).astype(np.float32)
    return c


def _col(g, n):
    return np.ascontiguousarray(np.asarray(g, np.float32).reshape(n, 128).T)


def _prep_shared(inp):
    f = lambda a: np.ascontiguousarray(np.asarray(a, dtype=np.float32))
    m = {}
    m["g0col"] = _col(inp["ab_norm_g"][0], 8)
    m["w_in0"] = f(inp["ab_w_in"][0])
    m["gq"] = f(np.tile(np.asarray(inp["sb_q_norm_g"][0]), 2)[:, None])
    m["gk"] = f(np.tile(np.asarray(inp["sb_k_norm_g"][0]), 2)[:, None])
    m["sgu_g"] = f(np.asarray(inp["sgu_norm_g"][0]).reshape(1, 512))
    m["sgu_wT"] = f(np.transpose(np.asarray(inp["sgu_w_s"][0]), (2, 0, 1)))
    m["sgu_b"] = f(np.asarray(inp["sgu_b_s"][0]).T)
    m["w_out0"] = f(inp["ab_w_out"][0])
    for l in range(2):
        m[f"g2col{l}"] = _col(inp["ffn_norm_g"][l], 8)
        we = np.transpose(np.asarray(inp["router_expert_w"][l]), (1, 0, 2)).reshape(1024, 16)
        m[f"wr{l}"] = f(np.concatenate([np.asarray(inp["router_group_w"][l]), we], axis=1))
        m[f"rb{l}"] = f(np.concatenate([np.asarray(inp["router_group_b"][l]), np.asarray(inp["router_expert_b"][l]).reshape(16)])[None, :])
        m[f"wg{l}"] = f(inp["expert_w_gate"][l])
        m[f"wu{l}"] = f(inp["expert_w_up"][l])
        m[f"wd{l}"] = f(inp["expert_w_down"][l])
    m["g1col"] = _col(inp["cd_norm_g"][0], 8)
    w1 = np.asarray(inp["cd_w_in"][0], np.float32)
    pad = np.zeros((1024, 56), np.float32)
    m["w_in1"] = f(np.concatenate([w1[:, 416:928], w1[:, 928:1440], w1[:, 1440:1952],
                                   w1[:, 0:256], w1[:, 256:384], w1[:, 384:416], w1[:, 2208:2240], w1[:, 2240:2248], pad,
                                   w1[:, 1952:2208]], axis=1))
    m["qlat_col"] = _col(inp["mla_q_latent_norm_g"][0], 2)
    m["kvlat_col"] = _col(inp["mla_kv_latent_norm_g"][0], 1)
    wuq = np.asarray(inp["mla_w_uq"][0], np.float32).reshape(256, 8, 96)
    m["w_uq"] = f(np.concatenate([wuq[:, :, 32:96], wuq[:, :, 0:32]], axis=2).reshape(256, 768))
    m["w_ukv"] = f(inp["mla_w_ukv"][0])
    qg = np.asarray(inp["mla_q_norm_g"][0], np.float32)
    m["mla_qg"] = f(np.concatenate([qg[32:96], qg[0:32]])[None, :])
    m["mla_kn_col"] = f(np.tile(np.asarray(inp["mla_k_nope_norm_g"][0]), 2)[:, None])
    m["mla_kr"] = f(np.asarray(inp["mla_k_rope_norm_g"][0])[None, :])
    m["dsa_qg"] = f(np.asarray(inp["dsa_q_norm_g"][0])[None, :])
    m["dsa_kg"] = f(np.asarray(inp["dsa_k_norm_g"][0])[None, :])
    m["w_out1"] = f(inp["cd_w_out"][0])
    return m


def _run(inp, S, stop="full", topk=256, n_cores=8):
    x = np.asarray(inp["x"], np.float32)
    pos = np.asarray(inp["positions"], np.int32)
    B = x.shape[0]
    NT = S // 128
    shared = _prep_shared(inp)
    shared.update(_consts(NT))
    nc = build(S, stop=stop, TOPK=topk)
    in_maps = []
    for c in range(n_cores):
        b = c % B
        m = dict(shared)
        m["x"] = np.ascontiguousarray(x[b])
        m["pos"] = np.ascontiguousarray(pos[b].reshape(NT, 128).T)
        in_maps.append(m)
    res = run_bass_kernel_spmd(nc, in_maps, core_ids=list(range(n_cores)))
    return np.stack([np.asarray(res.results[b]["out"], np.float32) for b in range(B)], axis=0)


def kernel(**inputs):
    return _run(inputs, 4096, "full", 256, 8)


def _phase_A1(L):
    g = L
    P = g["P"]; sb = g["sb"]; ps = g["ps"]; bc = g["bc"]; NT = g["NT"]; S = g["S"]
    ident_b = g["ident_b"]; eps_t = g["eps_t"]; cosT = g["cosT"]; sinT = g["sinT"]; sgnT = g["sgnT"]
    norm_tile = g["norm_tile"]; rstd_of = g["rstd_of"]
    with ExitStack() as es:
        W = sb(es, "W1", [128, 8, 2304], BF16)
        Wd = [Dep() for _ in range(8)]
        for c in range(8):
            P.dma("pool", W[:, c, :], g["w_in1"][c * 128:(c + 1) * 128, :], w=[Wd[c]])
        Wuq = sb(es, "Wuq", [128, 2, 768], BF16)
        P.dma("pool", Wuq[:], g["w_uq_d"].rearrange("(c p) n -> p c n", p=128), w=[Wuq])
        Wukv = sb(es, "Wukv", [128, 1024], BF16)
        P.dma("pool", Wukv[:], g["w_ukv_d"], w=[Wukv])
        gc = sb(es, "g1c", [128, 8]); P.dma("sp", gc[:], g["g1col"], w=[gc])
        qlc = sb(es, "qlc", [128, 2]); P.dma("sp", qlc[:], g["qlat_col"], w=[qlc])
        kvc = sb(es, "kvc", [128, 1]); P.dma("sp", kvc[:], g["kvlat_col"], w=[kvc])
        knc = sb(es, "knc", [128, 1]); P.dma("sp", knc[:], g["mla_kn_col"], w=[knc])
        qg96 = sb(es, "qg96", [128, 96]); P.dma("sp", qg96[:], g["mla_qg_d"].partition_broadcast(128), w=[qg96])
        kr32 = sb(es, "kr32", [128, 32]); P.dma("sp", kr32[:], g["mla_kr_d"].partition_broadcast(128), w=[kr32])
        dqg = sb(es, "dqg", [128, 64]); P.dma("sp", dqg[:], g["dsa_qg_d"].partition_broadcast(128), w=[dqg])
        dkg = sb(es, "dkg", [128, 64]); P.dma("sp", dkg[:], g["dsa_kg_d"].partition_broadcast(128), w=[dkg])
        NB = 2
        xt = Ring([sb(es, f"xt{i}", [128, 1024]) for i in range(NB)])
        junk = sb(es, "junk", [128, 1024], BF16)
        ss = Ring([sb(es, f"ss{i}", [128, 1]) for i in range(NB)])
        sd = Ring([sb(es, f"sd{i}", [128, 8]) for i in range(NB)])
        rstd = Ring([sb(es, f"rstd{i}", [128, 1]) for i in range(NB)])
        xn = Ring([sb(es, f"xn{i}", [128, 1024], BF16) for i in range(NB)])
        xnT = Ring([sb(es, f"xnT{i}", [128, 8, 128], BF16) for i in range(NB)])
        qTt = Ring([sb(es, f"qTt{i}", [128, 4, 128], BF16) for i in range(4)])
        vb = Ring([sb(es, f"vb{i}", [128, 512], BF16) for i in range(2)])
        qcT = Ring([sb(es, f"qcT{i}", [96, 8, 128], BF16) for i in range(2)])
        vcb = Ring([sb(es, f"vcb{i}", [128, 8, 64], BF16) for i in range(2)])
        kcT = Ring([sb(es, f"kcT{i}", [64, 8, 128], BF16) for i in range(2)])
        krT = Ring([sb(es, f"krT{i}", [32, 128], BF16) for i in range(2)])
        ikT = Ring([sb(es, f"ikTt{i}", [64, 128], BF16) for i in range(2)])
        aTt = Ring([sb(es, f"aTt{i}", [64, 4, 128], BF16) for i in range(2)])

        def two(name, shape, dt=F32):
            return [sb(es, f"{name}_{p}", shape, dt) for p in range(2)]
        qs2 = [two(f"qs{p}", [128, 512]) for p in range(2)]
        qg2 = [two(f"qg{p}", [128, 512]) for p in range(2)]
        qr2 = [two(f"qr{p}", [128, 512], BF16) for p in range(2)]
        sq2 = two("sq", [128, 768]); ssq2 = two("ssq", [128, 8]); rq2 = two("rq", [128, 8])
        ta2 = two("ta", [128, 128]); tb2 = two("tb", [128, 128])
        Lt2 = two("Lt", [128, 512])
        cqn2 = two("cqn", [128, 256], BF16); cqT2 = two("cqT", [128, 2, 128], BF16)
        qc2 = two("qc", [128, 768]); qcg2 = two("qcg", [128, 768]); qcr2 = two("qcr", [128, 768], BF16)
        ckn2 = two("ckn", [128, 128], BF16); ckT2 = two("ckT", [128, 128], BF16)
        kv2 = two("kv", [128, 1024]); knb2 = two("knb", [128, 8, 64], BF16)
        kpn2 = two("kpn", [128, 32]); kpr2 = two("kpr", [128, 32], BF16)
        iqf2 = two("iqf", [128, 256]); iqr2 = two("iqr", [128, 256]); ab2 = two("ab", [128, 256], BF16)
        ikr2 = two("ikr", [128, 32]); ikb2 = two("ikb", [128, 64], BF16)
        aw2 = two("aw", [128, 8])
        pT = ps(es, "pT1", [128, 1024], BF16)
        pQd = ps(es, "pQd", [128, 512]); pKd = ps(es, "pKd", [128, 512]); pVd = ps(es, "pVd", [128, 512])
        pL = ps(es, "pL", [128, 512]); pIq = ps(es, "pIq", [128, 512]); p2 = ps(es, "p2", [128, 1024])

        def tile_gen(t):
            pp_ = t % 2
            qs = Ring(qs2[pp_]); qg_ = Ring(qg2[pp_]); qr = Ring(qr2[pp_])
            sq = sq2[pp_]; ssq = ssq2[pp_]; rq = rq2[pp_]; ta = ta2[pp_]; tb = tb2[pp_]
            Lt = Lt2[pp_]; cqn = cqn2[pp_]; cqT = cqT2[pp_]; qc = qc2[pp_]; qcg = qcg2[pp_]; qcr = qcr2[pp_]
            ckn = ckn2[pp_]; ckT = ckT2[pp_]; kv = kv2[pp_]; knb = knb2[pp_]; kpn = kpn2[pp_]; kpr = kpr2[pp_]
            iqf = iqf2[pp_]; iqr = iqr2[pp_]; ab = ab2[pp_]; ikr = ikr2[pp_]; ikb = ikb2[pp_]; aw = aw2[pp_]

            def rotary(src, dst, H, half, lo, coff, t, r, w):
                cs = bc(cosT[:, t, coff:coff + half].unsqueeze(1), [128, H, half])
                sn = bc(sinT[:, t, coff:coff + half].unsqueeze(1), [128, H, half])
                x1 = src[:, :, lo:lo + half]; x2 = src[:, :, lo + half:lo + 2 * half]
                ta_ = ta[:, 0:H * half].rearrange("p (h d) -> p h d", h=H); tb_ = tb[:, 0:H * half].rearrange("p (h d) -> p h d", h=H)
                P.tt("dve", ta_, x1, cs, ALU.mult, r=r + [cosT], w=[ta])
                P.tt("dve", tb_, x2, sn, ALU.mult, r=r + [sinT], w=[tb])
                P.tt("dve", dst[:, :, lo:lo + half], ta_, tb_, ALU.subtract, r=[ta, tb], w=w)
                P.tt("dve", ta_, x2, cs, ALU.mult, r=r + [cosT], w=[ta])
                P.tt("dve", tb_, x1, sn, ALU.mult, r=r + [sinT], w=[tb])
                P.tt("dve", dst[:, :, lo + half:lo + 2 * half], ta_, tb_, ALU.add, r=[ta, tb], w=w)

            def hrstd(src_view, nh, dh, r, sdt):
                sqv = sq[:, 0:nh * dh].rearrange("p (h d) -> p h d", h=nh)
                P.tt("pool", sqv, src_view, src_view, ALU.mult, r=r, w=[sq])
                P.reduce(ssq[:, 0:nh], sqv, ALU.add, r=[sq], w=[ssq])
                rstd_of(None, ssq[:, 0:nh], float(dh), nh, sdt, rq, [ssq], None)

            o = (xt.get(t), junk, ss.get(t), sd.get(t), rstd.get(t), xn.get(t))
            xT = xnT.get(t)
            sdt = sd.get(t)
            norm_tile(o, g["h1_s"], t, gc, xT, pT)
            yield
            for n, pt_, wdt in ((0, pQd, 512), (1, pKd, 512), (2, pVd, 512), (3, pL, 512), (4, pIq, 256)):
                for c in range(8):
                    P.mm(pt_[:, 0:wdt], xT[:, c, :], W[:, c, n * 512:n * 512 + wdt], c == 0, c == 7, r=[xT, Wd[c]], w=[pt_])
            v_ = vb.get(t)
            P.copy("act", qs.get(0)[:], pQd[:], r=[pQd], w=[qs.get(0)])
            P.copy("act", qs.get(1)[:], pKd[:], r=[pKd], w=[qs.get(1)])
            P.copy("act", v_[:], pVd[:], r=[pVd], w=[v_])
            P.copy("act", Lt[:], pL[:], r=[pL], w=[Lt])
            P.copy("act", iqf[:], pIq[:, 0:256], r=[pIq], w=[iqf])
            P.copy("act", iqr[:], pIq[:, 0:256], r=[pIq], w=[iqr])
            yield
            for which, gt, dst in ((0, dqg, g["qTd_s"]), (1, dkg, g["kTd_s"])):
                q_ = qs.get(which); qgg = qg_.get(which); qr_ = qr.get(which); qT_ = qTt.get(2 * t + which)
                q3 = q_[:].rearrange("p (h d) -> p h d", h=8)
                hrstd(q3, 8, 64, [q_], sdt)
                g3 = qgg[:].rearrange("p (h d) -> p h d", h=8)
                P.tt("dve", g3, q3, bc(rq[:, 0:8].unsqueeze(2), [128, 8, 64]), ALU.mult, r=[q_, rq], w=[qgg])
                P.tt("pool", g3, g3, bc(gt[:, :].unsqueeze(1), [128, 8, 64]), ALU.mult, r=[qgg, gt], w=[qgg])
                yield
                r3 = qr_[:].rearrange("p (h d) -> p h d", h=8)
                P.copy("act", qr_[:], qgg[:], r=[qgg], w=[qr_])
                rotary(g3, r3, 8, 8, 0, 16, t, [qgg], [qr_])
                yield
                for i in range(4):
                    P.tr(pT[:, i * 128:(i + 1) * 128], qr_[:, i * 128:(i + 1) * 128], ident_b[:], r=[qr_, ident_b], w=[pT])
                P.copy("act", qT_[:], pT[:, 0:512].rearrange("p (i t) -> p i t", i=4), r=[pT], w=[qT_])
                P.dma("sp", dst[:, :, t * 128:(t + 1) * 128].rearrange("i p t -> p i t"), qT_[:], r=[qT_])
                yield
            P.dma("sp", g["vd_s"][t * 128:(t + 1) * 128, :], v_[:], r=[v_])
            hrstd(Lt[:, 0:256].rearrange("p (h d) -> p h d", h=1), 1, 256, [Lt], sdt)
            P.ts("dve", cqn[:], Lt[:, 0:256], rq[:, 0:1], None, ALU.mult, None, r=[Lt, rq], w=[cqn])
            yield
            for c in range(2):
                P.tr(pT[:, c * 128:(c + 1) * 128], cqn[:, c * 128:(c + 1) * 128], ident_b[:], r=[cqn, ident_b], w=[pT])
            P.tt("dve", cqT[:], pT[:, 0:256].rearrange("p (c t) -> p c t", c=2), bc(qlc[:, :].unsqueeze(2), [128, 2, 128]),
                 ALU.mult, r=[pT, qlc], w=[cqT])
            yield
            for n0, n1 in ((0, 512), (512, 768)):
                for c in range(2):
                    P.mm(p2[:, n0:n1], cqT[:, c, :], Wuq[:, c, n0:n1], c == 0, c == 1, r=[cqT, Wuq], w=[p2])
            P.copy("act", qc[:], p2[:, 0:768], r=[p2], w=[qc])
            yield
            c3 = qc[:].rearrange("p (h d) -> p h d", h=8)
            hrstd(c3, 8, 96, [qc], sdt)
            cg3 = qcg[:].rearrange("p (h d) -> p h d", h=8)
            P.tt("dve", cg3, c3, bc(rq[:, 0:8].unsqueeze(2), [128, 8, 96]), ALU.mult, r=[qc, rq], w=[qcg])
            P.tt("pool", cg3, cg3, bc(qg96[:, :].unsqueeze(1), [128, 8, 96]), ALU.mult, r=[qcg, qg96], w=[qcg])
            yield
            P.copy("act", qcr[:], qcg[:], r=[qcg], w=[qcr])
            rotary(cg3, qcr[:].rearrange("p (h d) -> p h d", h=8), 8, 16, 64, 0, t, [qcg], [qcr])
            yield
            for h in range(8):
                P.tr(pT[0:96, h * 128:(h + 1) * 128], qcr[:, h * 96:(h + 1) * 96], ident_b[:], r=[qcr, ident_b], w=[pT])
            qcT_ = qcT.get(t)
            P.copy("act", qcT_[:], pT[0:96, :].rearrange("p (h t) -> p h t", h=8), r=[pT], w=[qcT_])
            P.dma("sp", g["qTc_s"][:, :, t * 128:(t + 1) * 128].rearrange("h p t -> p h t"), qcT_[:], r=[qcT_])
            yield
            hrstd(Lt[:, 256:384].rearrange("p (h d) -> p h d", h=1), 1, 128, [Lt], sdt)
            P.ts("dve", ckn[:], Lt[:, 256:384], rq[:, 0:1], None, ALU.mult, None, r=[Lt, rq], w=[ckn])
            yield
            P.tr(pT[:, 0:128], ckn[:], ident_b[:], r=[ckn, ident_b], w=[pT])
            P.ts("dve", ckT[:], pT[:, 0:128], kvc[:, 0:1], None, ALU.mult, None, r=[pT, kvc], w=[ckT])
            yield
            for n in range(2):
                P.mm(p2[:, n * 512:(n + 1) * 512], ckT[:], Wukv[:, n * 512:(n + 1) * 512], True, True, r=[ckT, Wukv], w=[p2])
            P.copy("act", kv[:], p2[:], r=[p2], w=[kv])
            yield
            kv3 = kv[:].rearrange("p (h d) -> p h d", h=8)
            hrstd(kv3[:, :, 0:64], 8, 64, [kv], sdt)
            P.tt("dve", knb[:], kv3[:, :, 0:64], bc(rq[:, 0:8].unsqueeze(2), [128, 8, 64]), ALU.mult, r=[kv, rq], w=[knb])
            vc_ = vcb.get(t)
            P.copy("pool", vc_[:], kv3[:, :, 64:128], r=[kv], w=[vc_])
            P.dma("sp", g["vc_s"][t * 128:(t + 1) * 128, :].rearrange("p (h d) -> p h d", h=8), vc_[:], r=[vc_])
            yield
            for h in range(8):
                P.tr(pT[0:64, h * 128:(h + 1) * 128], knb[:, h, :], ident_b[:], r=[knb, ident_b], w=[pT])
            kcT_ = kcT.get(t)
            P.ts("dve", kcT_[:], pT[0:64, :].rearrange("p (h t) -> p h t", h=8), knc[0:64, 0:1], None, ALU.mult, None,
                 r=[pT, knc], w=[kcT_])
            P.dma("sp", g["kTc_s"][:, 0:64, t * 128:(t + 1) * 128].rearrange("h p t -> p h t"), kcT_[:], r=[kcT_])
            yield
            hrstd(Lt[:, 384:416].rearrange("p (h d) -> p h d", h=1), 1, 32, [Lt], sdt)
            P.ts("dve", kpn[:], Lt[:, 384:416], rq[:, 0:1], None, ALU.mult, None, r=[Lt, rq], w=[kpn])
            P.tt("dve", kpn[:], kpn[:], kr32[:], ALU.mult, r=[kpn, kr32], w=[kpn])
            yield
            rotary(kpn[:].rearrange("p (h d) -> p h d", h=1), kpr[:].rearrange("p (h d) -> p h d", h=1), 1, 16, 0, 0, t, [kpn], [kpr])
            yield
            P.tr(pT[0:32, 0:128], kpr[:], ident_b[:], r=[kpr, ident_b], w=[pT])
            krT_ = krT.get(t)
            P.copy("act", krT_[:], pT[0:32, 0:128], r=[pT], w=[krT_])
            for h in range(8):
                P.dma("sp", g["kTc_s"][h, 64:96, t * 128:(t + 1) * 128], krT_[:], r=[krT_])
            yield
            rotary(iqf[:].rearrange("p (h d) -> p h d", h=8), iqr[:].rearrange("p (h d) -> p h d", h=8), 8, 4, 0, 24, t, [iqf], [iqr])
            yield
            P.copy("pool", ikr[:], Lt[:, 416:448], r=[Lt], w=[ikr])
            rotary(Lt[:, 416:448].rearrange("p (h d) -> p h d", h=1), ikr[:].rearrange("p (h d) -> p h d", h=1), 1, 4, 0, 24, t, [Lt], [ikr])
            P.copy("pool", ikb[:, 0:32], ikr[:], r=[ikr], w=[ikb])
            P.copy("pool", ikb[:, 32:64], ikr[:], r=[ikr], w=[ikb])
            yield
            P.tr(pT[0:64, 0:128], ikb[:], ident_b[:], r=[ikb, ident_b], w=[pT])
            ikT_ = ikT.get(t)
            P.copy("act", ikT_[:], pT[0:64, 0:128], r=[pT], w=[ikT_])
            P.dma("sp", g["ikT_s"][:, t * 128:(t + 1) * 128], ikT_[:], r=[ikT_])
            yield
            P.act(sgnT[:, t, :], Lt[:, 448:456], AF.Sign, r=[Lt], w=[sgnT])
            P.stt("dve", aw[:], Lt[:, 448:456], -1.0, Lt[:, 448:456], ALU.mult, ALU.max, r=[Lt], w=[aw])
            P.ts("dve", aw[:], aw[:], 1.0 / 16.0, None, ALU.mult, None, r=[aw], w=[aw])
            P.tt("dve", ab[:].rearrange("p (h d) -> p h d", h=8), iqr[:].rearrange("p (h d) -> p h d", h=8),
                 bc(aw[:, 0:8].unsqueeze(2), [128, 8, 32]), ALU.mult, r=[iqr, aw], w=[ab])
            yield
            for i in range(4):
                P.tr(pT[0:64, i * 128:(i + 1) * 128], ab[:, i * 64:(i + 1) * 64], ident_b[:], r=[ab, ident_b], w=[pT])
            aT_ = aTt.get(t)
            P.copy("act", aT_[:], pT[0:64, 0:512].rearrange("p (i t) -> p i t", i=4), r=[pT], w=[aT_])
            P.dma("sp", g["aT_s"][:, :, t * 128:(t + 1) * 128].rearrange("i p t -> p i t"), aT_[:], r=[aT_])

        g["run_rolling"](tile_gen, NT, 13)
    P.barrier()
    stop = g["stop"]
    attention = g["attention"]; phase_CD = g["phase_CD"]
    if stop == "a1":
        return
    attention("mla", g["qTc_s"], g["kTc_s"], g["vc_s"], 0, 96.0 ** -0.5)
    if stop == "mla":
        return
    attention("dsa", g["qTd_s"], g["kTd_s"], g["vd_s"], 512, 0.125)
    if stop == "dsa":
        return
    phase_CD(1, g["h1_s"], g["out_d"], g["w_out1"])


phase_A1_holder[0] = _phase_A1
```

```python
import math
from contextlib import ExitStack
import numpy as np
import concourse.bass as bass
import concourse.mybir as mybir
from concourse.bass_utils import run_bass_kernel_spmd

F32 = mybir.dt.float32
BF16 = mybir.dt.bfloat16
I32 = mybir.dt.int32
AF = mybir.ActivationFunctionType
ALU = mybir.AluOpType
AX = mybir.AxisListType

EPOCH = 20000
D_MODEL = 1024
NEG = -30000.0
NBIS = 24
NOINT = False
TWO_PI = 2.0 * math.pi
CW1 = 6.28125
CW2 = 0.00193500518798828125
CW3 = TWO_PI - CW1 - CW2


class Dep:
    __slots__ = ("w", "rs")

    def __init__(self):
        self.w = None
        self.rs = []


class T:
    def __init__(self, t):
        self.t = t
        self.d = Dep()

    def __getitem__(self, k):
        return self.t[k]


class Op:
    __slots__ = ("eng", "fn", "waits", "idx", "chan", "cnt", "is_dma")


def _dep(x):
    return x.d if isinstance(x, T) else x


class Prog:
    ENGS = ("pe", "act", "dve", "pool", "sp")

    def __init__(self, nc, n_chan=40):
        self.nc = nc
        self.ops = {e: [] for e in self.ENGS}
        self.nidx = {e: 0 for e in self.ENGS}
        self.known = {e: {} for e in self.ENGS}
        self.chan_cnt = [0] * n_chan
        self.n_chan = n_chan
        self.rr = 0
        self.rr_sw = 0
        self.n_sw = 8
        self.pending = {e: [] for e in self.ENGS}

    def _wait(self, op, key, val):
        kn = self.known[op.eng]
        if kn.get(key, 0) >= val:
            return
        kn[key] = val
        op.waits = [w for w in op.waits if w[0] != key]
        op.waits.append((key, val))

    def _need(self, op, src):
        if src is None:
            return
        if src.is_dma:
            self._wait(op, ("c", src.chan), src.cnt)
        else:
            if src.eng == op.eng and op.eng == "pe":
                return
            self._wait(op, ("e", src.eng), src.idx + 1)

    def op(self, eng, fn, r=(), w=(), dma=False):
        o = Op()
        o.eng = eng
        o.fn = fn
        o.waits = []
        o.is_dma = dma
        for key, val in self.pending[eng]:
            self._wait(o, key, val)
        self.pending[eng] = []
        if dma:
            if eng == "pool":
                ch = self.n_chan - self.n_sw + self.rr_sw
                self.rr_sw = (self.rr_sw + 1) % self.n_sw
            else:
                ch = self.rr
                self.rr = (self.rr + 1) % (self.n_chan - self.n_sw)
            o.chan = ch
            if self.chan_cnt[ch] > 0:
                self._wait(o, ("c", ch), self.chan_cnt[ch])
            self.chan_cnt[ch] += 16
            o.cnt = self.chan_cnt[ch]
            o.idx = None
        else:
            o.chan = None
            o.cnt = None
            o.idx = self.nidx[eng]
            self.nidx[eng] += 1
        r = [_dep(x) for x in r]
        w = [_dep(x) for x in w]
        for d in r:
            self._need(o, d.w)
        for d in w:
            self._need(o, d.w)
            for q in d.rs:
                self._need(o, q)
        for d in r:
            d.rs.append(o)
        for d in w:
            d.w = o
            d.rs = []
        self.ops[eng].append(o)
        return o

    def barrier(self):
        waits = []
        for e in ("pe", "act", "dve", "pool"):
            if self.nidx[e] > 0:
                waits.append((("e", e), self.nidx[e]))
        for c in range(self.n_chan):
            if self.chan_cnt[c] > 0:
                waits.append((("c", c), self.chan_cnt[c]))
        for e in self.ENGS:
            self.pending[e] = list(waits)

    def dma(self, q, out, in_, r=(), w=()):
        return self.op(q, lambda e: e.dma_start(out=out, in_=in_), r, w, dma=True)

    def act(self, out, in_, func, r, w, **kw):
        return self.op("act", lambda e: e.activation(out=out, in_=in_, func=func, **kw), r, w)

    def tt(self, eng, out, in0, in1, op, r, w):
        return self.op(eng, lambda e: e.tensor_tensor(out=out, in0=in0, in1=in1, op=op), r, w)

    def ts(self, eng, out, in0, s1, s2, op0, op1, r, w, accum_out=None):
        if op1 is None:
            return self.op(eng, lambda e: e.tensor_scalar(out=out, in0=in0, scalar1=s1, scalar2=None, op0=op0), r, w)
        return self.op(eng, lambda e: e.tensor_scalar(out=out, in0=in0, scalar1=s1, scalar2=s2, op0=op0, op1=op1,
                                                      accum_out=accum_out), r, w)

    def stt(self, eng, out, in0, scalar, in1, op0, op1, r, w):
        return self.op(eng, lambda e: e.scalar_tensor_tensor(out=out, in0=in0, scalar=scalar, in1=in1, op0=op0, op1=op1), r, w)

    def mm(self, out, lhsT, rhs, start, stop, r, w):
        return self.op("pe", lambda e: e.matmul(out, lhsT=lhsT, rhs=rhs, start=start, stop=stop), r, w)

    def tr(self, out, in_, ident, r, w):
        return self.op("pe", lambda e: e.transpose(out, in_, ident), r, w)

    def copy(self, eng, out, in_, r, w):
        if eng == "act":
            return self.op("act", lambda e: e.copy(out=out, in_=in_), r, w)
        return self.op(eng, lambda e: e.tensor_copy(out=out, in_=in_), r, w)

    def memset(self, eng, ap, val, w):
        return self.op(eng, lambda e: e.memset(ap, val), (), w)

    def reduce(self, out, in_, op, r, w, axis=None):
        ax = AX.X if axis is None else axis
        return self.op("dve", lambda e: e.tensor_reduce(out=out, in_=in_, axis=ax, op=op), r, w)

    def recip(self, out, in_, r, w):
        return self.op("dve", lambda e: e.reciprocal(out=out, in_=in_), r, w)

    def emit(self):
        nc = self.nc
        with ExitStack() as es:
            esem = {e: [es.enter_context(nc.semaphore(f"s_{e}_{i}"))
                        for i in range(max((self.nidx[e] + EPOCH - 1) // EPOCH, 1))]
                    for e in ("pe", "act", "dve", "pool")}
            csem = [es.enter_context(nc.semaphore(f"c_{i}")) for i in range(self.n_chan)]
            block = es.enter_context(nc.Block())

            def do_waits(e, waits):
                for key, val in waits:
                    if key[0] == "c":
                        e.wait_ge(csem[key[1]], val)
                    else:
                        idx = val - 1
                        ep = idx // EPOCH
                        e.wait_ge(esem[key[1]][ep], idx - ep * EPOCH + 1)

            def run(e, name):
                for o in self.ops[name]:
                    do_waits(e, o.waits)
                    ins = o.fn(e)
                    if o.is_dma:
                        ins.then_inc(csem[o.chan], 16)
                    else:
                        ins.then_inc(esem[name][o.idx // EPOCH], 1)

            @block.tensor
            def _(e):
                run(e, "pe")

            @block.scalar
            def _(e):
                run(e, "act")

            @block.vector
            def _(e):
                run(e, "dve")

            @block.gpsimd
            def _(e):
                run(e, "pool")

            @block.sync
            def _(e):
                run(e, "sp")
                fin = []
                for c in range(self.n_chan):
                    if self.chan_cnt[c] > 0:
                        fin.append((("c", c), self.chan_cnt[c]))
                for en in ("pe", "act", "dve", "pool"):
                    if self.nidx[en] > 0:
                        fin.append((("e", en), self.nidx[en]))
                do_waits(e, fin)


class Ring:
    def __init__(self, items):
        self.items = items

    def get(self, i):
        return self.items[i % len(self.items)]


def build(S, stop="full", TOPK=256):
    NT = S // 128
    NG = S // 512
    SBT = min(8, NT)
    NSB = NT // SBT
    nc = bass.Bass("TRN2", target_bir_lowering=False)
    P = Prog(nc)

    def din(name, shape, dt=F32):
        return nc.dram_tensor(name, list(shape), dt, kind="ExternalInput").ap()

    def dscr(name, shape, dt):
        return nc.dram_tensor(name, list(shape), dt, kind="Internal").ap()

    x_d = din("x", [S, 1024])
    pos_d = din("pos", [128, NT], I32)
    c_ident = din("c_ident", [128, 128])
    c_sgumask = din("c_sgumask", [128, 128])
    c_negsb = din("c_negsb", [4, 128, 512])
    c_negcc = din("c_negcc", [4, 128, 512])
    c_adm = din("c_adm", [128, 128])
    c_invf = din("c_invf", [128, 28])
    c_pow2 = din("c_pow2", [128, NBIS + 1])
    c_tri = din("c_tri", [128, 128])
    c_trii = din("c_trii", [128, 128])
    g0col = din("g0col", [128, 8])
    w_in0 = din("w_in0", [1024, 2560])
    gq_d = din("gq", [128, 1])
    gk_d = din("gk", [128, 1])
    sgu_g_d = din("sgu_g", [1, 512])
    sgu_wT_d = din("sgu_wT", [128, 8, 128])
    sgu_b_d = din("sgu_b", [128, 8])
    w_out0 = din("w_out0", [1024, 1024])
    g2col = [din(f"g2col{l}", [128, 8]) for l in range(2)]
    wr_d = [din(f"wr{l}", [1024, 20]) for l in range(2)]
    rb_d = [din(f"rb{l}", [1, 20]) for l in range(2)]
    wg_d = [din(f"wg{l}", [16, 1024, 512]) for l in range(2)]
    wu_d = [din(f"wu{l}", [16, 1024, 512]) for l in range(2)]
    wd_d = [din(f"wd{l}", [16, 512, 1024]) for l in range(2)]
    g1col = din("g1col", [128, 8])
    w_in1 = din("w_in1", [1024, 2304])
    qlat_col = din("qlat_col", [128, 2])
    kvlat_col = din("kvlat_col", [128, 1])
    w_uq_d = din("w_uq", [256, 768])
    w_ukv_d = din("w_ukv", [128, 1024])
    mla_qg_d = din("mla_qg", [1, 96])
    mla_kn_col = din("mla_kn_col", [128, 1])
    mla_kr_d = din("mla_kr", [1, 32])
    dsa_qg_d = din("dsa_qg", [1, 64])
    dsa_kg_d = din("dsa_kg", [1, 64])
    w_out1 = din("w_out1", [1024, 1024])
    out_d = nc.dram_tensor("out", [S, 1024], F32, kind="ExternalOutput").ap()

    qT0_s = dscr("qT0_s", [4, 128, S], BF16)
    kT0_s = dscr("kT0_s", [4, 128, S], BF16)
    v0_s = dscr("v0_s", [S, 512], BF16)
    mixed_s = dscr("mixed_s", [S, 1024], BF16)
    h1_s = dscr("h1_s", [S, 1024], F32)
    qTc_s = dscr("qTc_s", [8, 96, S], BF16)
    kTc_s = dscr("kTc_s", [8, 96, S], BF16)
    vc_s = dscr("vc_s", [S, 512], BF16)
    qTd_s = dscr("qTd_s", [4, 128, S], BF16)
    kTd_s = dscr("kTd_s", [4, 128, S], BF16)
    vd_s = dscr("vd_s", [S, 512], BF16)
    aT_s = dscr("aT_s", [4, 64, S], BF16)
    ikT_s = dscr("ikT_s", [64, S], BF16)
    NMT_d = dscr("NMT_d", [NG, NT, 128, 512], BF16)
    hmid_s = dscr("hmid_s", [S, 1024], F32)

    top = ExitStack()
    with top:
        uniq = [0]

        def sb(es, name, shape, dt=F32):
            uniq[0] += 1
            return T(es.enter_context(nc.sbuf_tensor(f"sb{uniq[0]}_{name}", list(shape), dt)))

        def ps(es, name, shape, dt=F32):
            uniq[0] += 1
            return T(es.enter_context(nc.psum_tensor(f"ps{uniq[0]}_{name}", list(shape), dt)))

        def bc(ap, shape):
            return ap.to_broadcast(list(shape))

        ident_f = sb(top, "ident_f", [128, 128])
        ident_b = sb(top, "ident_b", [128, 128], BF16)
        eps_t = sb(top, "eps_t", [128, 1])
        cosT = sb(top, "cosT", [128, NT, 28])
        sinT = sb(top, "sinT", [128, NT, 28])
        sgnT = sb(top, "sgnT", [128, NT, 8])
        P.dma("sp", ident_f[:], c_ident, w=[ident_f])
        P.dma("pool", ident_b[:], c_ident, w=[ident_b])
        P.memset("dve", eps_t[:], 1e-6, w=[eps_t])

        def rstd_of(es_tag, ss_ap, n_el, width, scr_sd, out_r, r, eng_w):
            P.act(scr_sd[:, 0:width], ss_ap, AF.Sqrt, r=r, w=[scr_sd], scale=1.0 / n_el, bias=eps_t[:, 0:1])
            P.recip(out_r[:, 0:width], scr_sd[:, 0:width], r=[scr_sd], w=[out_r])

        def run_rolling(gen_fn, n_tiles, K):
            for t0 in range(0, n_tiles, K):
                gens = [gen_fn(t0 + i) for i in range(min(K, n_tiles - t0))]
                while gens:
                    for gg in list(gens):
                        try:
                            next(gg)
                        except StopIteration:
                            gens.remove(gg)

        def phase_R():
            with ExitStack() as es:
                pi_ = sb(es, "pos_i", [128, NT], I32)
                pf = sb(es, "pos_f", [128, NT])
                invf = sb(es, "invf", [128, 28])
                ang = sb(es, "ang", [128, NT, 28])
                tq = sb(es, "tq", [128, NT, 28])
                nq = sb(es, "nq", [128, NT, 28])
                rr = sb(es, "rr", [128, NT, 28])
                P.dma("sp", pi_[:], pos_d, w=[pi_])
                P.dma("sp", invf[:], c_invf, w=[invf])
                P.copy("dve", pf[:], pi_[:], r=[pi_], w=[pf])
                P.tt("dve", ang[:], bc(pf[:, :].unsqueeze(2), [128, NT, 28]), bc(invf[:, :].unsqueeze(1), [128, NT, 28]),
                     ALU.mult, r=[pf, invf], w=[ang])
                P.ts("dve", tq[:], ang[:], 1.0 / TWO_PI, None, ALU.mult, None, r=[ang], w=[tq])
                P.ts("dve", nq[:], tq[:], 12582912.0, None, ALU.add, None, r=[tq], w=[nq])
                P.ts("dve", tq[:], nq[:], 12582912.0, None, ALU.subtract, None, r=[nq], w=[tq])
                P.stt("dve", rr[:], tq[:], -CW1, ang[:], ALU.mult, ALU.add, r=[tq, ang], w=[rr])
                P.stt("dve", nq[:], tq[:], -CW2, rr[:], ALU.mult, ALU.add, r=[tq, rr], w=[nq])
                P.stt("dve", rr[:], tq[:], -CW3, nq[:], ALU.mult, ALU.add, r=[tq, nq], w=[rr])
                P.ts("dve", rr[:], rr[:], 3.14159, -3.14159, ALU.min, ALU.max, r=[rr], w=[rr])
                P.act(sinT[:], rr[:], AF.Sin, r=[rr], w=[sinT])
                P.stt("dve", nq[:], rr[:], -1.0, rr[:], ALU.mult, ALU.max, r=[rr], w=[nq])
                P.ts("dve", tq[:], nq[:], -1.0, math.pi / 2, ALU.mult, ALU.add, r=[nq], w=[tq])
                P.act(cosT[:], tq[:], AF.Sin, r=[tq], w=[cosT])
            P.barrier()

        def norm_tile(es_objs, src_d, t, gcol, xnT, pT):
            xt, junk, ss, sd, rstd, xn = es_objs
            P.dma("sp", xt[:], src_d[t * 128:(t + 1) * 128, :], w=[xt])
            P.act(junk[:], xt[:], AF.Square, r=[xt], w=[junk, ss], accum_out=ss[:, 0:1])
            rstd_of(None, ss[:, 0:1], 1024.0, 1, sd, rstd, [ss], None)
            P.ts("dve", xn[:], xt[:], rstd[:, 0:1], None, ALU.mult, None, r=[xt, rstd], w=[xn])
            for c in range(8):
                P.tr(pT[:, c * 128:(c + 1) * 128], xn[:, c * 128:(c + 1) * 128], ident_b[:], r=[xn, ident_b], w=[pT])
            P.tt("dve", xnT[:], pT[:].rearrange("p (c t) -> p c t", c=8), bc(gcol[:, :].unsqueeze(2), [128, 8, 128]),
                 ALU.mult, r=[pT, gcol], w=[xnT])

        def head_rstd(src_f32, nh, dh, sq, ssq, sd, rq, r):
            P.tt("pool", sq[:, 0:nh * dh], src_f32, src_f32, ALU.mult, r=r, w=[sq])
            P.reduce(ssq[:, 0:nh], sq[:, 0:nh * dh].rearrange("p (h d) -> p h d", h=nh), ALU.add, r=[sq], w=[ssq])
            rstd_of(None, ssq[:, 0:nh], float(dh), nh, sd, rq, [ssq], None)

        def phase_A0():
            with ExitStack() as es:
                W = sb(es, "W0", [128, 8, 2560], BF16)
                Wd = [Dep() for _ in range(8)]
                for c in range(8):
                    P.dma("pool", W[:, c, :], w_in0[c * 128:(c + 1) * 128, :], w=[Wd[c]])
                gc = sb(es, "g0c", [128, 8])
                P.dma("sp", gc[:], g0col, w=[gc])
                gq = sb(es, "gq", [128, 1]); gk = sb(es, "gk", [128, 1])
                P.dma("sp", gq[:], gq_d, w=[gq]); P.dma("sp", gk[:], gk_d, w=[gk])
                sgg = sb(es, "sgg", [128, 512])
                P.dma("sp", sgg[:], sgu_g_d.partition_broadcast(128), w=[sgg])
                sgb = sb(es, "sgb", [128, 8])
                P.dma("sp", sgb[:], sgu_b_d, w=[sgb])
                wsf = sb(es, "wsf", [128, 8, 128]); msk = sb(es, "msk", [128, 128])
                WsT = sb(es, "WsT", [128, 8, 128], BF16)
                P.dma("sp", wsf[:], sgu_wT_d, w=[wsf]); P.dma("sp", msk[:], c_sgumask, w=[msk])
                P.tt("dve", WsT[:], wsf[:], bc(msk[:, :].unsqueeze(1), [128, 8, 128]), ALU.mult, r=[wsf, msk], w=[WsT])
                KA0 = 3
                NB = KA0
                xt = Ring([sb(es, f"xt{i}", [128, 1024]) for i in range(NB)])
                junk = sb(es, "junk", [128, 1024], BF16)
                ss = Ring([sb(es, f"ss{i}", [128, 1]) for i in range(NB)])
                sd = Ring([sb(es, f"sd{i}", [128, 8]) for i in range(NB)])
                rstd = Ring([sb(es, f"rstd{i}", [128, 1]) for i in range(NB)])
                xn = Ring([sb(es, f"xn{i}", [128, 1024], BF16) for i in range(NB)])
                xnT = Ring([sb(es, f"xnT{i}", [128, 8, 128], BF16) for i in range(NB)])
                def two0(name, shape, dt=F32):
                    return [sb(es, f"{name}_{p}", shape, dt) for p in range(KA0)]
                qs2 = [[sb(es, f"qs{p}_{w}", [128, 512]) for w in range(2)] for p in range(KA0)]
                qn2 = [[sb(es, f"qn{p}_{w}", [128, 512], BF16) for w in range(2)] for p in range(KA0)]
                sq2 = two0("sq", [128, 512]); ssq2 = two0("ssq", [128, 8]); rq2 = two0("rq", [128, 8])
                qTt = Ring([sb(es, f"qTt{i}", [128, 4, 128], BF16) for i in range(2 * KA0)])
                vb = Ring([sb(es, f"vb{i}", [128, 512], BF16) for i in range(KA0)])
                xs2 = two0("xs", [128, 1024]); x22 = two0("x2", [128, 1024]); sg2 = two0("sg", [128, 1024])
                gl2 = two0("gl", [128, 1024])
                zn12 = two0("zn1", [128, 512]); znb2 = two0("zn", [128, 512], BF16)
                mb2 = two0("mb", [128, 512]); bo = Ring([sb(es, f"bo{i}", [128, 512], BF16) for i in range(KA0)])
                pT = ps(es, "pT", [128, 1024], BF16)
                pQ = ps(es, "pQ", [128, 512]); pK = ps(es, "pK", [128, 512]); pV = ps(es, "pV", [128, 512])
                pUZ = ps(es, "pUZ", [128, 1024]); pM = ps(es, "pM", [128, 512])

                def tile_gen0(t):
                    pp_ = t % KA0
                    qs = Ring(qs2[pp_]); qn = Ring(qn2[pp_])
                    sq = sq2[pp_]; ssq = ssq2[pp_]; rq = rq2[pp_]
                    xs = xs2[pp_]; x2 = x22[pp_]; sg = sg2[pp_]; gl = gl2[pp_]
                    zn1 = zn12[pp_]; zn = znb2[pp_]; mb = mb2[pp_]
                    o = (xt.get(t), junk, ss.get(t), sd.get(t), rstd.get(t), xn.get(t))
                    xT = xnT.get(t)
                    norm_tile(o, x_d, t, gc, xT, pT)
                    yield
                    for n, pt_ in enumerate((pQ, pK, pV)):
                        for c in range(8):
                            P.mm(pt_[:, :], xT[:, c, :], W[:, c, n * 512:(n + 1) * 512], c == 0, c == 7, r=[xT, Wd[c]], w=[pt_])
                    for n in range(2):
                        for c in range(8):
                            P.mm(pUZ[:, n * 512:(n + 1) * 512], xT[:, c, :], W[:, c, (3 + n) * 512:(4 + n) * 512], c == 0, c == 7,
                                 r=[xT, Wd[c]], w=[pUZ])
                    v_ = vb.get(t)
                    P.copy("act", qs.get(0)[:], pQ[:], r=[pQ], w=[qs.get(0)])
                    P.copy("act", qs.get(1)[:], pK[:], r=[pK], w=[qs.get(1)])
                    P.copy("act", v_[:], pV[:], r=[pV], w=[v_])
                    P.copy("act", xs[:], pUZ[:], r=[pUZ], w=[xs])
                    yield
                    for which, gcol_, dst in ((0, gq, qT0_s), (1, gk, kT0_s)):
                        q_ = qs.get(which); qn_ = qn.get(which); qT_ = qTt.get(2 * t + which)
                        head_rstd(q_[:], 8, 64, sq, ssq, sd.get(t), rq, [q_])
                        P.tt("dve", qn_[:].rearrange("p (h d) -> p h d", h=8), q_[:].rearrange("p (h d) -> p h d", h=8),
                             bc(rq[:, 0:8].unsqueeze(2), [128, 8, 64]), ALU.mult, r=[q_, rq], w=[qn_])
                        yield
                        for i in range(4):
                            P.tr(pT[:, i * 128:(i + 1) * 128], qn_[:, i * 128:(i + 1) * 128], ident_b[:], r=[qn_, ident_b], w=[pT])
                        P.ts("dve", qT_[:], pT[:, 0:512].rearrange("p (i t) -> p i t", i=4), gcol_[:, 0:1],
                             0.125 if which == 0 else 1.0, ALU.mult, ALU.mult, r=[pT, gcol_], w=[qT_])
                        P.dma("sp", dst[:, :, t * 128:(t + 1) * 128].rearrange("i p t -> p i t"), qT_[:], r=[qT_])
                        yield
                    P.dma("sp", v0_s[t * 128:(t + 1) * 128, :], v_[:], r=[v_])
                    P.tt("pool", x2[:], xs[:], xs[:], ALU.mult, r=[xs], w=[x2])
                    P.ts("pool", x2[:], x2[:], 0.044715, 1.0, ALU.mult, ALU.add, r=[x2], w=[x2])
                    P.tt("pool", x2[:], x2[:], xs[:], ALU.mult, r=[x2, xs], w=[x2])
                    yield
                    P.act(sg[:], x2[:], AF.Sigmoid, r=[x2], w=[sg], scale=1.5957691216057308)
                    P.tt("pool", gl[:], sg[:], xs[:], ALU.mult, r=[sg, xs], w=[gl])
                    yield
                    head_rstd(gl[:, 512:1024], 8, 64, sq, ssq, sd.get(t), rq, [gl])
                    P.tt("dve", zn1[:].rearrange("p (h d) -> p h d", h=8), gl[:, 512:1024].rearrange("p (h d) -> p h d", h=8),
                         bc(rq[:, 0:8].unsqueeze(2), [128, 8, 64]), ALU.mult, r=[gl, rq], w=[zn1])
                    P.tt("pool", zn[:], zn1[:], sgg[:], ALU.mult, r=[zn1, sgg], w=[zn])
                    yield
                    for g in range(8):
                        P.mm(pM[:, g * 64:(g + 1) * 64], WsT[:, g, :], zn[:, g * 64:(g + 1) * 64], True, True, r=[WsT, zn], w=[pM])
                    P.tt("dve", mb[:].rearrange("p (h d) -> p h d", h=8), pM[:].rearrange("p (h d) -> p h d", h=8),
                         bc(sgb[:, 0:8].unsqueeze(2), [128, 8, 64]), ALU.add, r=[pM, sgb], w=[mb])
                    bo_ = bo.get(t)
                    P.tt("pool", bo_[:], mb[:], gl[:, 0:512], ALU.mult, r=[mb, gl], w=[bo_])
                    P.dma("sp", mixed_s[t * 128:(t + 1) * 128, 512:1024], bo_[:], r=[bo_])

                run_rolling(tile_gen0, NT, KA0)
            P.barrier()

        def attention(kind, qT_d, kT_d, v_d, col0, scale):
            mla = kind == "mla"
            DP = 96 if mla else 128
            NP = 8 if mla else 4
            D = 96 if mla else 64
            with ExitStack() as es:
                kT = sb(es, "kT", [DP, NP, S], BF16)
                kTd = [Dep() for _ in range(NP)]
                for i in range(NP):
                    P.dma("sp", kT[:, i, :], kT_d[i], w=[kTd[i]])
                vS = sb(es, "vS", [128, NT, 8, 65], BF16)
                P.memset("pool", vS[:], 1.0, w=[vS])
                for t0 in range(NT):
                    P.dma("sp", vS[:, t0, :, 0:64], v_d[t0 * 128:(t0 + 1) * 128, :].rearrange("p (h d) -> p h d", h=8), w=[vS])
                negm = sb(es, "negm", [128, 4, 512], BF16)
                if kind != "dsa":
                    src = c_negsb if kind == "sb" else c_negcc
                    P.dma("pool", negm[:], src.rearrange("j p q -> p j q"), w=[negm])
                qTg = Ring([sb(es, f"qTg{i}", [DP, NP, 512], BF16) for i in range(2)])
                oS = Ring([sb(es, f"oS{i}", [128, 4, 512], BF16) for i in range(2)])
                pt = Ring([sb(es, f"pt{i}", [128, 512], BF16) for i in range(5)])
                rec = Ring([sb(es, f"rec{i}", [128, 4]) for i in range(2)])
                psc = Ring([ps(es, f"psc{i}", [128, 512]) for i in range(3)])
                pacc = Ring([ps(es, f"pacc{i}", [128, 512]) for i in range(2)])
                if kind == "sb":
                    tri = sb(es, "tri", [128, 128], BF16); onesn = sb(es, "onesn", [128, 128], BF16)
                    P.dma("pool", tri[:], c_tri, w=[tri])
                    P.memset("pool", onesn[:], -1.0, w=[onesn])
                    eS = Ring([sb(es, f"eS{i}", [128, 512]) for i in range(2)])
                    spS = Ring([sb(es, f"spS{i}", [128, 512]) for i in range(2)])
                    spB = Ring([sb(es, f"spB{i}", [128, 512], BF16) for i in range(2)])
                    t1S = Ring([sb(es, f"t1S{i}", [128, 512]) for i in range(2)])
                    lacc = sb(es, "lacc", [128, 512]); laccB = Ring([sb(es, f"laccB{i}", [128, 512], BF16) for i in range(2)])
                    plat = Ring([ps(es, f"plat{i}", [128, 512]) for i in range(2)])
                if kind == "dsa":
                    ikT = sb(es, "ikT", [64, S], BF16)
                    P.dma("sp", ikT[:], ikT_s, w=[ikT])
                    aTg = Ring([sb(es, f"aTg{i}", [64, 4, 512], BF16) for i in range(2)])
                    score = sb(es, "score", [128, S])
                    NM = sb(es, "NM", [128, S], BF16)
                    NMT = sb(es, "NMT", [128, NT, 512], BF16)
                    Rb = Ring([sb(es, f"Rb{i}", [128, 512]) for i in range(4)])
                    accA = sb(es, "accA", [128, 512]); accB = sb(es, "accB", [128, 512])
                    admb = sb(es, "admb", [128, 128]); pw2 = sb(es, "pw2", [128, NBIS + 1])
                    P.dma("sp", admb[:], c_adm, w=[admb]); P.dma("sp", pw2[:], c_pow2, w=[pw2])
                    dtab = sb(es, "dtab", [128, NBIS + 1]); dtab2 = sb(es, "dtab2", [128, NBIS + 1])
                    mn = sb(es, "mn", [128, 1]); mx = sb(es, "mx", [128, 1]); d0 = sb(es, "d0", [128, 1])
                    mid = sb(es, "mid", [128, 1]); cnt = sb(es, "cnt", [128, 1]); gp = sb(es, "gp", [128, 1])
                    tau = sb(es, "tau", [128, 1]); cjunk = sb(es, "cjunk", [128, S], BF16)
                    plog = Ring([ps(es, f"plog{i}", [128, 512]) for i in range(2)])
                    pTm = ps(es, "pTm", [128, 1024], BF16)
                    pTmd = [Dep(), Dep()]
                    NMTd = [Dep() for _ in range(NT)]
                    stg = Ring([sb(es, f"stg{i}", [128, 4, 128], BF16) for i in range(4)])
                    P.memset("pool", NMT[:], NEG, w=NMTd)
                    nmt_w = {}
                    grpc = [0]

                    def prep_thunks(G):
                        th = []
                        a_ = aTg.get(G)
                        th.append(lambda: P.dma("sp", a_[:], aT_s[:, :, G * 512:(G + 1) * 512].rearrange("i p t -> p i t"), w=[a_]))
                        for j in range(4):
                            qb = 4 * G + j
                            nvis = (qb + 1) * 128
                            sc_ = score[:, 0:nvis]
                            for kc in range(0, nvis, 512):
                                wdt = min(512, nvis - kc)
                                for h in range(8):
                                    def f_idx(h=h, kc=kc, wdt=wdt, qb=qb, j=j):
                                        pl_ = plog.get(h)
                                        b0 = (h % 2) * 32
                                        P.mm(pl_[:, 0:wdt], a_[b0:b0 + 32, h // 2, j * 128:(j + 1) * 128], ikT[b0:b0 + 32, kc:kc + wdt],
                                             True, True, r=[a_, ikT], w=[pl_])
                                        R_ = Rb.get(h)
                                        P.act(R_[:, 0:wdt], pl_[:, 0:wdt], AF.Relu, r=[pl_], w=[R_])
                                        acc = accA if h < 4 else accB
                                        sg_ = sgnT[:, qb, h:h + 1]
                                        if h % 4 == 0:
                                            P.ts("dve", acc[:, 0:wdt], R_[:, 0:wdt], sg_, None, ALU.mult, None, r=[R_, sgnT], w=[acc])
                                        else:
                                            P.stt("dve", acc[:, 0:wdt], R_[:, 0:wdt], sg_, acc[:, 0:wdt], ALU.mult, ALU.add,
                                                  r=[R_, sgnT, acc], w=[acc])
                                    th.append(f_idx)
                                th.append(lambda kc=kc, wdt=wdt: P.tt("dve", score[:, kc:kc + wdt], accA[:, 0:wdt], accB[:, 0:wdt], ALU.add,
                                                                      r=[accA, accB], w=[score]))

                            def f_init(qb=qb, nvis=nvis, sc_=sc_):
                                P.reduce(mn[:], sc_, ALU.min, r=[score], w=[mn])
                                P.reduce(mx[:], sc_, ALU.max, r=[score], w=[mx])
                                P.tt("dve", score[:, qb * 128:nvis], score[:, qb * 128:nvis], admb[:], ALU.add, r=[score, admb], w=[score])
                                P.tt("dve", d0[:], mx[:], mn[:], ALU.subtract, r=[mx, mn], w=[d0])
                                P.ts("dve", d0[:], d0[:], 1.001, 1e-6, ALU.mult, ALU.add, r=[d0], w=[d0])
                                P.ts("dve", dtab[:], pw2[:], d0[:, 0:1], None, ALU.mult, None, r=[pw2, d0], w=[dtab])
                                P.ts("dve", dtab2[:], dtab[:], 2.0, None, ALU.mult, None, r=[dtab], w=[dtab2])
                                P.tt("dve", mid[:], mn[:], dtab[:, 0:1], ALU.add, r=[mn, dtab], w=[mid])
                            th.append(f_init)
                            for k in range(NBIS):
                                def f_bis(k=k, nvis=nvis, sc_=sc_):
                                    P.ts("dve", cjunk[:, 0:nvis], sc_, mid[:, 0:1], None, ALU.is_ge, ALU.add, r=[score, mid], w=[cjunk, cnt],
                                         accum_out=cnt[:, 0:1])
                                    P.stt("dve", gp[:], cnt[:], float(TOPK) - 0.5, dtab2[:, k + 1:k + 2], ALU.is_ge, ALU.mult,
                                          r=[cnt, dtab2], w=[gp])
                                    P.stt("dve", mid[:], mid[:], dtab[:, k + 1:k + 2], gp[:], ALU.subtract, ALU.add, r=[mid, dtab, gp], w=[mid])
                                th.append(f_bis)

                            def f_fin(nvis=nvis, sc_=sc_):
                                P.tt("dve", tau[:], mid[:], dtab2[:, NBIS:NBIS + 1], ALU.subtract, r=[mid, dtab2], w=[tau])
                                P.ts("dve", NM[:, 0:nvis], sc_, tau[:, 0:1], NEG, ALU.is_lt, ALU.mult, r=[score, tau], w=[NM])
                            th.append(f_fin)
                            for k0 in range(0, qb + 1, 4):
                                def f_tr(k0=k0, qb=qb, j=j):
                                    k1 = min(k0 + 3, qb)
                                    n_ = k1 - k0 + 1
                                    par = 0
                                    stg_ = stg.get(grpc[0])
                                    grpc[0] += 1
                                    for kt in range(k0, k1 + 1):
                                        c0 = par * 512 + (kt - k0) * 128
                                        P.tr(pTm[:, c0:c0 + 128], NM[:, kt * 128:(kt + 1) * 128], ident_b[:],
                                             r=[NM, ident_b], w=[pTm])
                                    P.copy("act", stg_[:, 0:n_, :],
                                           pTm[:, par * 512:par * 512 + n_ * 128].rearrange("p (k q) -> p k q", k=n_), r=[pTm], w=[stg_])
                                    dd = Dep()
                                    nmt_w[(G, j, k0 // 4)] = dd
                                    P.dma("sp", NMT_d[G, k0:k1 + 1, :, j * 128:(j + 1) * 128].rearrange("k p q -> p k q"), stg_[:, 0:n_, :],
                                          r=[stg_], w=[dd])
                                th.append(f_tr)
                        return th
                ui = 0
                for G in range(NG):
                    q_ = qTg.get(G)
                    P.dma("sp", q_[:], qT_d[:, :, G * 512:(G + 1) * 512].rearrange("i p t -> p i t"), w=[q_])
                    nkt = 4 * G + 4
                    nxt = []
                    if kind == "dsa":
                        if G == 0:
                            for f in prep_thunks(0):
                                f()
                        if G + 1 < NG:
                            nxt = prep_thunks(G + 1)
                            if NOINT:
                                for f in nxt:
                                    f()
                                nxt = []
                        for kt in range(nkt):
                            j0 = max(0, kt - 4 * G)
                            P.dma("sp", NMT[:, kt, j0 * 128:512], NMT_d[G, kt, :, j0 * 128:512],
                                  r=[nmt_w[(G, j, kt // 4)] for j in range(j0, 4)], w=[NMTd[kt]])
                    o_ = oS.get(G)
                    units = [(h, kt) for h in range(8) for kt in range(nkt)]
                    ui0 = ui
                    pts = {}

                    def emit_QK(u):
                        h, kt = units[u]
                        pr = h if mla else h // 2
                        b0 = 0 if mla else (h % 2) * 64
                        sc = psc.get(ui0 + u)
                        diag = kt >= 4 * G
                        masked = diag or kind == "dsa"
                        P.mm(sc[:], kT[b0:b0 + D, pr, kt * 128:(kt + 1) * 128], q_[b0:b0 + D, pr, :], True, not masked,
                             r=[kTd[pr], q_], w=[sc])
                        if masked:
                            if kind == "dsa":
                                P.mm(sc[:], ident_b[:], NMT[:, kt, :], False, True, r=[ident_b, NMTd[kt]], w=[sc])
                            else:
                                P.mm(sc[:], ident_b[:], negm[:, kt - 4 * G, :], False, True, r=[ident_b, negm], w=[sc])
                        p_ = pt.get(ui0 + u)
                        P.act(p_[:], sc[:], AF.Exp, r=[sc], w=[p_], scale=scale)
                        pts[u] = p_

                    def emit_PV(u):
                        h, kt = units[u]
                        accT = pacc.get(G * 8 + h)
                        acc = accT[:, 0:260].rearrange("p (j c) -> p j c", j=4)
                        p_ = pts.pop(u)
                        js = [j for j in range(4) if kt <= 4 * G + j]
                        for j in js:
                            P.mm(acc[:, j, :], p_[:, j * 128:(j + 1) * 128], vS[:, kt, h, :], kt == 0 and j == js[0],
                                 kt == nkt - 1 and j == js[-1], r=[p_, vS], w=[accT])
                        if kt == nkt - 1:
                            rec_ = rec.get(G * 8 + h)
                            P.recip(rec_[:], acc[:, :, 64], r=[accT], w=[rec_])
                            P.tt("dve", o_[:, :, h * 64:(h + 1) * 64], acc[:, :, 0:64], bc(rec_[:, 0:4].unsqueeze(2), [128, 4, 64]),
                                 ALU.mult, r=[accT, rec_], w=[o_])

                    emit_QK(0)
                    emit_QK(1)
                    ti = 0
                    for u in range(len(units)):
                        if u + 2 < len(units):
                            emit_QK(u + 2)
                        emit_PV(u)
                        tgt = (u + 1) * len(nxt) // len(units)
                        while ti < tgt:
                            nxt[ti]()
                            ti += 1
                    ui += len(units)
                    P.dma("sp", mixed_s[G * 512:(G + 1) * 512, col0:col0 + 512].rearrange("(j p) c -> p j c", p=128), o_[:], r=[o_])
            P.barrier()

        def attention_sb(qT_d, kT_d, v_d, col0):
            with ExitStack() as es:
                kT = sb(es, "kT", [128, 4, S], BF16)
                kTd = [Dep() for _ in range(4)]
                for i in range(4):
                    P.dma("sp", kT[:, i, :], kT_d[i], w=[kTd[i]])
                vS = sb(es, "vS", [128, NT, 8, 64], BF16)
                for t0 in range(NT):
                    P.dma("sp", vS[:, t0, :, :], v_d[t0 * 128:(t0 + 1) * 128, :].rearrange("p (h d) -> p h d", h=8), w=[vS])
                negm = sb(es, "negm", [128, 4, 512], BF16)
                P.dma("pool", negm[:], c_negsb.rearrange("j p q -> p j q"), w=[negm])
                trii = sb(es, "trii", [128, 128], BF16); onesn = sb(es, "onesn", [128, 128], BF16)
                P.dma("pool", trii[:], c_trii, w=[trii])
                P.memset("dve", onesn[:], -1.0, w=[onesn])
                qTg = Ring([sb(es, f"qTg{i}", [128, 4, 512], BF16) for i in range(2)])
                oS = Ring([sb(es, f"oS{i}", [128, 4, 512], BF16) for i in range(2)])
                eS = Ring([sb(es, f"eS{i}", [128, 512]) for i in range(4)])
                spB = Ring([sb(es, f"spB{i}", [128, 512], BF16) for i in range(8)])
                pt = Ring([sb(es, f"pt{i}", [128, 512], BF16) for i in range(6)])
                lacc = [sb(es, f"lacc{i}", [128, 512]) for i in range(2)]
                laccB = [Ring([sb(es, f"laccB{i}_{k}", [128, 512], BF16) for k in range(4)]) for i in range(2)]
                psZ = Ring([ps(es, f"psZ{i}", [128, 512]) for i in range(3)])
                psL = Ring([ps(es, f"psL{i}", [128, 512]) for i in range(3)])
                pacc = [ps(es, f"pacc{i}", [128, 512]) for i in range(2)]
                steps = []
                for G in range(NG):
                    nkt = 4 * G + 4
                    for hp in range(4):
                        for i in range(nkt):
                            steps.append((G, hp, i, nkt))
                st = {}
                qs_ = {}
                uzc = [0]

                def stage1(s):
                    G, hp, i, nkt = steps[s]
                    for Gl in ([0] if s == 0 else []) + ([G + 1] if (hp == 1 and i == 0 and G + 1 < NG) else []):
                        ql = qTg.get(Gl)
                        P.dma("sp", ql[:], qT_d[:, :, Gl * 512:(Gl + 1) * 512].rearrange("i p t -> p i t"), w=[ql])
                        qs_[Gl] = ql
                    q_ = qs_[G]
                    for hs in range(2):
                        b0 = hs * 64
                        kt = nkt - 1 - i
                        diag = kt >= 4 * G
                        uz = 2 * s + hs
                        Z = psZ.get(uz); e_ = eS.get(uz); sp_ = spB.get(uz)
                        P.mm(Z[:], kT[b0:b0 + 64, hp, kt * 128:(kt + 1) * 128], q_[b0:b0 + 64, hp, :], True, not diag,
                             r=[kTd[hp], q_], w=[Z])
                        if diag:
                            P.mm(Z[:], ident_b[:], negm[:, kt - 4 * G, :], False, True, r=[ident_b, negm], w=[Z])
                        P.act(e_[:], Z[:], AF.Exp, r=[Z], w=[e_])
                        P.act(sp_[:], e_[:], AF.Ln, r=[e_], w=[sp_], bias=1.0)
                        lb = laccB[hs].get(s)
                        if i == 0:
                            P.copy("dve", lb[:], sp_[:], r=[sp_], w=[lb])
                            P.copy("dve", lacc[hs][:], sp_[:], r=[sp_], w=[lacc[hs]])
                        elif i < nkt - 1:
                            P.tt("dve", lb[:], lacc[hs][:], sp_[:], ALU.add, r=[lacc[hs], sp_], w=[lb])
                            P.tt("dve", lacc[hs][:], lacc[hs][:], sp_[:], ALU.add, r=[lacc[hs], sp_], w=[lacc[hs]])
                        st[(hs, s)] = [sp_, lb, kt, diag, None]

                def stage2(s):
                    G, hp, u, nkt = steps[s]
                    q_ = qs_[G]
                    for hs in range(2):
                        b0 = hs * 64
                        sp_, _, kt, diag, _ = st[(hs, s)]
                        uz = 2 * s + hs
                        Lp = psL.get(uz); p_ = pt.get(uz)
                        mms = [(kT[b0:b0 + 64, hp, kt * 128:(kt + 1) * 128], q_[b0:b0 + 64, hp, :], [kTd[hp], q_]),
                               (trii[:], sp_[:], [trii, sp_])]
                        if u > 0:
                            lbp = st[(hs, s - 1)][1]
                            mms.append((onesn[:], lbp[:], [onesn, lbp]))
                        if diag:
                            mms.append((ident_b[:], negm[:, kt - 4 * G, :], [ident_b, negm]))
                        for mi, (l_, r_, rd) in enumerate(mms):
                            P.mm(Lp[:], l_, r_, mi == 0, mi == len(mms) - 1, r=rd, w=[Lp])
                        P.act(p_[:], Lp[:], AF.Exp, r=[Lp], w=[p_])
                        st[(hs, s)][4] = p_

                def stage3(s):
                    G, hp, u, nkt = steps[s]
                    o_ = oS.get(G)
                    for hs in range(2):
                        h = 2 * hp + hs
                        _, _, kt, diag, p_ = st[(hs, s)]
                        accT = pacc[hs]
                        acc = accT[:, 0:256].rearrange("p (j c) -> p j c", j=4)
                        js = [j for j in range(4) if kt <= 4 * G + j]
                        for j in js:
                            P.mm(acc[:, j, :], p_[:, j * 128:(j + 1) * 128], vS[:, kt, h, :], u == 0 and j == js[0],
                                 u == nkt - 1 and j == js[-1], r=[p_, vS], w=[accT])
                        if u == nkt - 1:
                            P.copy("act", o_[:, :, h * 64:(h + 1) * 64], accT[:, 0:256].rearrange("p (j c) -> p j c", j=4),
                                   r=[accT], w=[o_])
                    if s >= 2:
                        st.pop((0, s - 2), None); st.pop((1, s - 2), None)
                    if u == nkt - 1 and hp == 3:
                        P.dma("sp", mixed_s[G * 512:(G + 1) * 512, col0:col0 + 512].rearrange("(j p) c -> p j c", p=128), o_[:], r=[o_])

                NS = len(steps)
                for s in range(NS + 2):
                    if s < NS:
                        stage1(s)
                    if 0 <= s - 1 < NS:
                        stage2(s - 1)
                    if 0 <= s - 2 < NS:
                        stage3(s - 2)
            P.barrier()

        def phase_CD(layer, hin_d, hout_d, w_out_d):
            with ExitStack() as es:
                Wo = sb(es, "Wo", [128, 8, 1024], BF16)
                Wod = [Dep() for _ in range(8)]
                for c in range(8):
                    P.dma("pool", Wo[:, c, :], w_out_d[c * 128:(c + 1) * 128, :], w=[Wod[c]])
                g2 = sb(es, "g2", [128, 8])
                P.dma("sp", g2[:], g2col[layer], w=[g2])
                Wr = sb(es, "Wr", [128, 8, 20])
                P.dma("sp", Wr[:], wr_d[layer].rearrange("(c p) n -> p c n", p=128), w=[Wr])
                rb = sb(es, "rb", [128, 20])
                P.dma("sp", rb[:], rb_d[layer].partition_broadcast(128), w=[rb])
                hS = [sb(es, f"hS{j}", [128, 1024]) for j in range(SBT)]
                hP = Ring([sb(es, f"hP{i}", [128, 1024]) for i in range(2)])
                hm_dep = [Dep() for _ in range(NT)]
                xTb = [sb(es, f"xTb{i}", [128, 8, SBT * 128], BF16) for i in range(2)]
                xTbd = [[Dep() for _ in range(SBT)] for _ in range(2)]
                gates = [[sb(es, f"gates{i}_{j}", [128, 16]) for j in range(SBT)] for i in range(2)]
                mx_ = Ring([sb(es, f"mxd{i}", [128, 1024], BF16) for i in range(2)])
                mT = Ring([sb(es, f"mT{i}", [128, 8, 128], BF16) for i in range(2)])
                junk = sb(es, "junkc", [128, 1024], BF16)
                ss = sb(es, "ssc", [128, 1]); sd = sb(es, "sdc", [128, 8]); rstd = sb(es, "rstdc", [128, 1])
                xn = sb(es, "xnc", [128, 1024]); xTf = sb(es, "xTf", [128, 8, 128])
                rl = sb(es, "rl", [128, 20]); m4 = sb(es, "m4", [128, 1]); e4 = sb(es, "e4", [128, 4]); s4 = sb(es, "s4", [128, 1])
                pg_ = sb(es, "pg_", [128, 1]); oh = sb(es, "oh", [128, 4]); tm = sb(es, "tm", [128, 16]); il = sb(es, "il", [128, 4])
                ex = sb(es, "ex", [128, 4]); o1 = sb(es, "o1", [128, 4]); ex2 = sb(es, "ex2", [128, 4]); m2 = sb(es, "m2", [128, 1])
                o2 = sb(es, "o2", [128, 4]); den = sb(es, "den", [128, 1]); gi = sb(es, "gi", [128, 4])
                wg = Ring([sb(es, f"wg{i}", [128, 8, 512], BF16) for i in range(2)])
                wu = Ring([sb(es, f"wu{i}", [128, 8, 512], BF16) for i in range(2)])
                wd = Ring([sb(es, f"wd{i}", [128, 4, 1024], BF16) for i in range(2)])
                sgs = Ring([sb(es, f"sgs{i}", [128, 512]) for i in range(4)])
                hid = Ring([sb(es, f"hid{i}", [128, 512], BF16) for i in range(4)])
                hT = Ring([sb(es, f"hT{i}", [128, 4, 128], BF16) for i in range(4)])
                pT = ps(es, "pTc", [128, 1024], BF16)
                pO = ps(es, "pO", [128, 1024])
                pGs = [ps(es, f"pG{i}", [128, 512]) for i in range(2)]
                pUs = [ps(es, f"pU{i}", [128, 512]) for i in range(2)]
                pX = pO
                pR = ps(es, "pR", [128, 512])

                def prep_thunks(sbi):
                    par = sbi % 2
                    th = []

                    def mk(sbi, j):
                        t = sbi * SBT + j
                        h_ = hP.get(t); m_ = mx_.get(t); mT_ = mT.get(t)

                        def T0():
                            P.dma("sp", m_[:], mixed_s[t * 128:(t + 1) * 128, :], w=[m_])
                            P.dma("sp", h_[:], hin_d[t * 128:(t + 1) * 128, :], w=[h_])

                        def T1():
                            for c in range(8):
                                P.tr(pT[:, c * 128:(c + 1) * 128], m_[:, c * 128:(c + 1) * 128], ident_b[:], r=[m_, ident_b], w=[pT])
                            P.copy("act", mT_[:], pT[:].rearrange("p (c t) -> p c t", c=8), r=[pT], w=[mT_])

                        def T2():
                            for n in range(2):
                                for c in range(8):
                                    P.mm(pO[:, n * 512:(n + 1) * 512], mT_[:, c, :], Wo[:, c, n * 512:(n + 1) * 512], c == 0, c == 7,
                                         r=[mT_, Wod[c]], w=[pO])
                            P.tt("dve", h_[:], pO[:], h_[:], ALU.add, r=[pO, h_], w=[h_])
                            P.dma("sp", hmid_s[t * 128:(t + 1) * 128, :], h_[:], r=[h_], w=[hm_dep[t]])

                        def T3():
                            P.act(junk[:], h_[:], AF.Square, r=[h_], w=[junk, ss], accum_out=ss[:, 0:1])
                            rstd_of(None, ss[:, 0:1], 1024.0, 1, sd, rstd, [ss], None)
                            P.ts("dve", xn[:], h_[:], rstd[:, 0:1], None, ALU.mult, None, r=[h_, rstd], w=[xn])

                        def T4():
                            for c in range(8):
                                P.tr(pX[:, c * 128:(c + 1) * 128], xn[:, c * 128:(c + 1) * 128], ident_f[:], r=[xn, ident_f], w=[pX])
                            P.tt("dve", xTf[:], pX[:].rearrange("p (c t) -> p c t", c=8), bc(g2[:, :].unsqueeze(2), [128, 8, 128]),
                                 ALU.mult, r=[pX, g2], w=[xTf])
                            P.copy("pool", xTb[par][:, :, j * 128:(j + 1) * 128], xTf[:], r=[xTf], w=[xTbd[par][j]])

                        def T5():
                            for c in range(8):
                                P.mm(pR[:, 0:20], xTf[:, c, :], Wr[:, c, :], c == 0, c == 7, r=[xTf, Wr], w=[pR])
                            P.tt("dve", rl[:], pR[:, 0:20], rb[:], ALU.add, r=[pR, rb], w=[rl])
                            P.reduce(m4[:], rl[:, 0:4], ALU.max, r=[rl], w=[m4])
                            P.ts("dve", oh[:], rl[:, 0:4], m4[:, 0:1], None, ALU.is_ge, None, r=[rl, m4], w=[oh])
                            P.ts("dve", e4[:], rl[:, 0:4], m4[:, 0:1], None, ALU.subtract, None, r=[rl, m4], w=[e4])
                            P.act(e4[:], e4[:], AF.Exp, r=[e4], w=[e4])
                            P.reduce(s4[:], e4[:], ALU.add, r=[e4], w=[s4])
                            P.recip(pg_[:], s4[:], r=[s4], w=[pg_])
                            P.tt("dve", tm[:].rearrange("p (g e) -> p g e", g=4), rl[:, 4:20].rearrange("p (g e) -> p g e", g=4),
                                 bc(oh[:, 0:4].unsqueeze(2), [128, 4, 4]), ALU.mult, r=[rl, oh], w=[tm])
                            P.reduce(il[:], tm[:].rearrange("p (g e) -> p e g", g=4), ALU.add, r=[tm], w=[il])
                            P.reduce(m4[:], il[:], ALU.max, r=[il], w=[m4])
                            P.ts("dve", ex[:], il[:], m4[:, 0:1], None, ALU.subtract, None, r=[il, m4], w=[ex])
                            P.act(ex[:], ex[:], AF.Exp, r=[ex], w=[ex])
                            P.ts("dve", o1[:], il[:], m4[:, 0:1], None, ALU.is_ge, None, r=[il, m4], w=[o1])
                            P.stt("dve", ex2[:], o1[:], -2.0, ex[:], ALU.mult, ALU.add, r=[o1, ex], w=[ex2])
                            P.reduce(m2[:], ex2[:], ALU.max, r=[ex2], w=[m2])
                            P.ts("dve", o2[:], ex2[:], m2[:, 0:1], None, ALU.is_ge, None, r=[ex2, m2], w=[o2])
                            P.ts("dve", den[:], m2[:], 1.0, None, ALU.add, None, r=[m2], w=[den])
                            P.recip(den[:], den[:], r=[den], w=[den])
                            P.tt("dve", den[:], den[:], pg_[:], ALU.mult, r=[den, pg_], w=[den])
                            P.tt("dve", o1[:], o1[:], o2[:], ALU.add, r=[o1, o2], w=[o1])
                            P.tt("dve", gi[:], o1[:], ex[:], ALU.mult, r=[o1, ex], w=[gi])
                            P.ts("dve", gi[:], gi[:], den[:, 0:1], None, ALU.mult, None, r=[gi, den], w=[gi])
                            P.tt("dve", gates[par][j][:].rearrange("p (g e) -> p g e", g=4), bc(oh[:, 0:4].unsqueeze(2), [128, 4, 4]),
                                 bc(gi[:, 0:4].unsqueeze(1), [128, 4, 4]), ALU.mult, r=[oh, gi], w=[gates[par][j]])
                        return T0, [T1, T2, T3, T4, T5]

                    parts = [mk(sbi, j) for j in range(SBT)]
                    th.append(parts[0][0])
                    for j in range(SBT):
                        if j + 1 < SBT:
                            th.append(parts[j + 1][0])
                        th.extend(parts[j][1])
                    return th

                for f in prep_thunks(0):
                    f()
                for sbi in range(NSB):
                    par = sbi % 2
                    nxt = prep_thunks(sbi + 1) if sbi + 1 < NSB else []
                    for j in range(SBT):
                        t = sbi * SBT + j
                        P.dma("sp", hS[j][:], hmid_s[t * 128:(t + 1) * 128, :], r=[hm_dep[t]], w=[hS[j]])
                    units = [(e, j) for e in range(16) for j in range(SBT)]
                    ubase = sbi * len(units)

                    def emit_GU(k):
                        e, j = units[k]
                        it = sbi * 16 + e
                        wg_ = wg.get(it); wu_ = wu.get(it); wd_ = wd.get(it)
                        if j == 0:
                            P.dma("pool", wg_[:], wg_d[layer][e].rearrange("(c p) n -> p c n", p=128), w=[wg_])
                            P.dma("pool", wu_[:], wu_d[layer][e].rearrange("(c p) n -> p c n", p=128), w=[wu_])
                            P.dma("pool", wd_[:], wd_d[layer][e].rearrange("(c p) n -> p c n", p=128), w=[wd_])
                        pG = pGs[k % 2]; pU = pUs[k % 2]
                        for c in range(8):
                            P.mm(pG[:], xTb[par][:, c, j * 128:(j + 1) * 128], wg_[:, c, :], c == 0, c == 7, r=[xTbd[par][j], wg_], w=[pG])
                        for c in range(8):
                            P.mm(pU[:], xTb[par][:, c, j * 128:(j + 1) * 128], wu_[:, c, :], c == 0, c == 7, r=[xTbd[par][j], wu_], w=[pU])
                        sg_ = sgs.get(ubase + k); hd_ = hid.get(ubase + k)
                        P.act(sg_[:], pG[:], AF.Silu, r=[pG], w=[sg_])
                        P.stt("dve", hd_[:], sg_[:], gates[par][j][:, e:e + 1], pU[:], ALU.mult, ALU.mult, r=[sg_, gates[par][j], pU], w=[hd_])

                    def emit_T(k):
                        hd_ = hid.get(ubase + k); hT_ = hT.get(ubase + k)
                        for c in range(4):
                            P.tr(pT[:, c * 128:(c + 1) * 128], hd_[:, c * 128:(c + 1) * 128], ident_b[:], r=[hd_, ident_b], w=[pT])
                        P.copy("act", hT_[:], pT[:, 0:512].rearrange("p (c t) -> p c t", c=4), r=[pT], w=[hT_])

                    def emit_D(k):
                        e, j = units[k]
                        it = sbi * 16 + e
                        wd_ = wd.get(it)
                        hT_ = hT.get(ubase + k)
                        for n in range(2):
                            for c in range(4):
                                P.mm(pO[:, n * 512:(n + 1) * 512], hT_[:, c, :], wd_[:, c, n * 512:(n + 1) * 512], c == 0, c == 3,
                                     r=[hT_, wd_], w=[pO])
                        P.tt("dve", hS[j][:], pO[:], hS[j][:], ALU.add, r=[pO, hS[j]], w=[hS[j]])

                    emit_GU(0)
                    emit_GU(1)
                    emit_T(0)
                    ti = 0
                    for k in range(len(units)):
                        if k + 2 < len(units):
                            emit_GU(k + 2)
                        if k + 1 < len(units):
                            emit_T(k + 1)
                        emit_D(k)
                        tgt = (k + 1) * len(nxt) // len(units)
                        while ti < tgt:
                            nxt[ti]()
                            ti += 1
                    for j in range(SBT):
                        t = sbi * SBT + j
                        P.dma("sp", hout_d[t * 128:(t + 1) * 128, :], hS[j][:], r=[hS[j]])
            P.barrier()

        phase_R()
        phase_A0()
        if stop != "a0":
            attention_sb(qT0_s, kT0_s, v0_s, 0)
        if stop in ("a0", "sb"):
            pass
        elif stop in ("mix0", "l0"):
            phase_CD(0, x_d, out_d, w_out0)
            P.barrier()
        else:
            phase_CD(0, x_d, h1_s, w_out0)
            phase_A1_holder[0](locals())
        P.emit()
    return nc


phase_A1_holder = [None]


def _consts(NT):
    c = {}
    c["c_ident"] = np.eye(128, dtype=np.float32)
    j = np.arange(128)
    c["c_sgumask"] = ((j[:, None] // 64) <= (j[None, :] // 64)).astype(np.float32)
    negsb = np.zeros((4, 128, 512), np.float32)
    negcc = np.zeros((4, 128, 512), np.float32)
    q = np.arange(512)
    for jj in range(4):
        s = jj * 128 + np.arange(128)
        negsb[jj] = np.where(s[:, None] < q[None, :], 0.0, NEG)
        negcc[jj] = np.where((s[:, None] // 64) <= (q[None, :] // 64), 0.0, NEG)
    c["c_negsb"] = negsb
    c["c_negcc"] = negcc
    c["c_adm"] = np.where((j[None, :] // 64) <= (j[:, None] // 64), 0.0, -1e30).astype(np.float32)
    fr = []
    for rot in (32, 16, 8):
        half = rot // 2
        fr.append(np.power(np.float32(500000.0), -np.arange(half, dtype=np.float32) * np.float32(2.0 / rot)).astype(np.float32))
    c["c_invf"] = np.tile(np.concatenate(fr)[None, :], (128, 1)).astype(np.float32)
    c["c_pow2"] = np.tile((2.0 ** -(np.arange(NBIS + 1) + 1.0))[None, :], (128, 1)).astype(np.float32)
    c["c_tri"] = np.where(j[:, None] > j[None, :], -1.0, 0.0).astype(np.float32)
    c["c_trii"] = np.where(j[:, None] >= j[None, :], -1.0, 0.0).astype(np.float32)
    return c


def _col(g, n):
    return np.ascontiguousarray(np.asarray(g, np.float32).reshape(n, 128).T)


def _prep_shared(inp):
    f = lambda a: np.ascontiguousarray(np.asarray(a, dtype=np.float32))
    m = {}
    m["g0col"] = _col(inp["ab_norm_g"][0], 8)
    m["w_in0"] = f(inp["ab_w_in"][0])
    m["gq"] = f(np.tile(np.asarray(inp["sb_q_norm_g"][0]), 2)[:, None])
    m["gk"] = f(np.tile(np.asarray(inp["sb_k_norm_g"][0]), 2)[:, None])
    m["sgu_g"] = f(np.asarray(inp["sgu_norm_g"][0]).reshape(1, 512))
    m["sgu_wT"] = f(np.transpose(np.asarray(inp["sgu_w_s"][0]), (2, 0, 1)))
    m["sgu_b"] = f(np.asarray(inp["sgu_b_s"][0]).T)
    m["w_out0"] = f(inp["ab_w_out"][0])
    for l in range(2):
        m[f"g2col{l}"] = _col(inp["ffn_norm_g"][l], 8)
        we = np.transpose(np.asarray(inp["router_expert_w"][l]), (1, 0, 2)).reshape(1024, 16)
        m[f"wr{l}"] = f(np.concatenate([np.asarray(inp["router_group_w"][l]), we], axis=1))
        m[f"rb{l}"] = f(np.concatenate([np.asarray(inp["router_group_b"][l]), np.asarray(inp["router_expert_b"][l]).reshape(16)])[None, :])
        m[f"wg{l}"] = f(inp["expert_w_gate"][l])
        m[f"wu{l}"] = f(inp["expert_w_up"][l])
        m[f"wd{l}"] = f(inp["expert_w_down"][l])
    m["g1col"] = _col(inp["cd_norm_g"][0], 8)
    w1 = np.asarray(inp["cd_w_in"][0], np.float32)
    pad = np.zeros((1024, 56), np.float32)
    m["w_in1"] = f(np.concatenate([w1[:, 416:928], w1[:, 928:1440], w1[:, 1440:1952],
                                   w1[:, 0:256], w1[:, 256:384], w1[:, 384:416], w1[:, 2208:2240], w1[:, 2240:2248], pad,
                                   w1[:, 1952:2208]], axis=1))
    m["qlat_col"] = _col(inp["mla_q_latent_norm_g"][0], 2)
    m["kvlat_col"] = _col(inp["mla_kv_latent_norm_g"][0], 1)
    wuq = np.asarray(inp["mla_w_uq"][0], np.float32).reshape(256, 8, 96)
    m["w_uq"] = f(np.concatenate([wuq[:, :, 32:96], wuq[:, :, 0:32]], axis=2).reshape(256, 768))
    m["w_ukv"] = f(inp["mla_w_ukv"][0])
    qg = np.asarray(inp["mla_q_norm_g"][0], np.float32)
    m["mla_qg"] = f(np.concatenate([qg[32:96], qg[0:32]])[None, :])
    m["mla_kn_col"] = f(np.tile(np.asarray(inp["mla_k_nope_norm_g"][0]), 2)[:, None])
    m["mla_kr"] = f(np.asarray(inp["mla_k_rope_norm_g"][0])[None, :])
    m["dsa_qg"] = f(np.asarray(inp["dsa_q_norm_g"][0])[None, :])
    m["dsa_kg"] = f(np.asarray(inp["dsa_k_norm_g"][0])[None, :])
    m["w_out1"] = f(inp["cd_w_out"][0])
    return m


def _run(inp, S, stop="full", topk=256, n_cores=8):
    x = np.asarray(inp["x"], np.float32)
    pos = np.asarray(inp["positions"], np.int32)
    B = x.shape[0]
    NT = S // 128
    shared = _prep_shared(inp)
    shared.update(_consts(NT))
    nc = build(S, stop=stop, TOPK=topk)
    in_maps = []
    for c in range(n_cores):
        b = c % B
        m = dict(shared)
        m["x"] = np.ascontiguousarray(x[b])
        m["pos"] = np.ascontiguousarray(pos[b].reshape(NT, 128).T)
        in_maps.append(m)
    res = run_bass_kernel_spmd(nc, in_maps, core_ids=list(range(n_cores)))
    return np.stack([np.asarray(res.results[b]["out"], np.float32) for b in range(B)], axis=0)


def kernel(**inputs):
    return _run(inputs, 4096, "full", 256, 8)


def _phase_A1(L):
    g = L
    P = g["P"]; sb = g["sb"]; ps = g["ps"]; bc = g["bc"]; NT = g["NT"]; S = g["S"]
    ident_b = g["ident_b"]; eps_t = g["eps_t"]; cosT = g["cosT"]; sinT = g["sinT"]; sgnT = g["sgnT"]
    norm_tile = g["norm_tile"]; rstd_of = g["rstd_of"]
    with ExitStack() as es:
        W = sb(es, "W1", [128, 8, 2304], BF16)
        Wd = [Dep() for _ in range(8)]
        for c in range(8):
            P.dma("pool", W[:, c, :], g["w_in1"][c * 128:(c + 1) * 128, :], w=[Wd[c]])
        Wuq = sb(es, "Wuq", [128, 2, 768], BF16)
        P.dma("pool", Wuq[:], g["w_uq_d"].rearrange("(c p) n -> p c n", p=128), w=[Wuq])
        Wukv = sb(es, "Wukv", [128, 1024], BF16)
        P.dma("pool", Wukv[:], g["w_ukv_d"], w=[Wukv])
        gc = sb(es, "g1c", [128, 8]); P.dma("sp", gc[:], g["g1col"], w=[gc])
        qlc = sb(es, "qlc", [128, 2]); P.dma("sp", qlc[:], g["qlat_col"], w=[qlc])
        kvc = sb(es, "kvc", [128, 1]); P.dma("sp", kvc[:], g["kvlat_col"], w=[kvc])
        knc = sb(es, "knc", [128, 1]); P.dma("sp", knc[:], g["mla_kn_col"], w=[knc])
        qg96 = sb(es, "qg96", [128, 96]); P.dma("sp", qg96[:], g["mla_qg_d"].partition_broadcast(128), w=[qg96])
        kr32 = sb(es, "kr32", [128, 32]); P.dma("sp", kr32[:], g["mla_kr_d"].partition_broadcast(128), w=[kr32])
        dqg = sb(es, "dqg", [128, 64]); P.dma("sp", dqg[:], g["dsa_qg_d"].partition_broadcast(128), w=[dqg])
        dkg = sb(es, "dkg", [128, 64]); P.dma("sp", dkg[:], g["dsa_kg_d"].partition_broadcast(128), w=[dkg])
        NB = 2
        xt = Ring([sb(es, f"xt{i}", [128, 1024]) for i in range(NB)])
        junk = sb(es, "junk", [128, 1024], BF16)
        ss = Ring([sb(es, f"ss{i}", [128, 1]) for i in range(NB)])
        sd = Ring([sb(es, f"sd{i}", [128, 8]) for i in range(NB)])
        rstd = Ring([sb(es, f"rstd{i}", [128, 1]) for i in range(NB)])
        xn = Ring([sb(es, f"xn{i}", [128, 1024], BF16) for i in range(NB)])
        xnT = Ring([sb(es, f"xnT{i}", [128, 8, 128], BF16) for i in range(NB)])
        qTt = Ring([sb(es, f"qTt{i}", [128, 4, 128], BF16) for i in range(4)])
        vb = Ring([sb(es, f"vb{i}", [128, 512], BF16) for i in range(2)])
        qcT = Ring([sb(es, f"qcT{i}", [96, 8, 128], BF16) for i in range(2)])
        vcb = Ring([sb(es, f"vcb{i}", [128, 8, 64], BF16) for i in range(2)])
        kcT = Ring([sb(es, f"kcT{i}", [64, 8, 128], BF16) for i in range(2)])
        krT = Ring([sb(es, f"krT{i}", [32, 128], BF16) for i in range(2)])
        ikT = Ring([sb(es, f"ikTt{i}", [64, 128], BF16) for i in range(2)])
        aTt = Ring([sb(es, f"aTt{i}", [64, 4, 128], BF16) for i in range(2)])

        def two(name, shape, dt=F32):
            return [sb(es, f"{name}_{p}", shape, dt) for p in range(2)]
        qs2 = [two(f"qs{p}", [128, 512]) for p in range(2)]
        qg2 = [two(f"qg{p}", [128, 512]) for p in range(2)]
        qr2 = [two(f"qr{p}", [128, 512], BF16) for p in range(2)]
        sq2 = two("sq", [128, 768]); ssq2 = two("ssq", [128, 8]); rq2 = two("rq", [128, 8])
        ta2 = two("ta", [128, 128]); tb2 = two("tb", [128, 128])
        Lt2 = two("Lt", [128, 512])
        cqn2 = two("cqn", [128, 256], BF16); cqT2 = two("cqT", [128, 2, 128], BF16)
        qc2 = two("qc", [128, 768]); qcg2 = two("qcg", [128, 768]); qcr2 = two("qcr", [128, 768], BF16)
        ckn2 = two("ckn", [128, 128], BF16); ckT2 = two("ckT", [128, 128], BF16)
        kv2 = two("kv", [128, 1024]); knb2 = two("knb", [128, 8, 64], BF16)
        kpn2 = two("kpn", [128, 32]); kpr2 = two("kpr", [128, 32], BF16)
        iqf2 = two("iqf", [128, 256]); iqr2 = two("iqr", [128, 256]); ab2 = two("ab", [128, 256], BF16)
        ikr2 = two("ikr", [128, 32]); ikb2 = two("ikb", [128, 64], BF16)
        aw2 = two("aw", [128, 8])
        pT = ps(es, "pT1", [128, 1024], BF16)
        pQd = ps(es, "pQd", [128, 512]); pKd = ps(es, "pKd", [128, 512]); pVd = ps(es, "pVd", [128, 512])
        pL = ps(es, "pL", [128, 512]); pIq = ps(es, "pIq", [128, 512]); p2 = ps(es, "p2", [128, 1024])

        def tile_gen(t):
            pp_ = t % 2
            qs = Ring(qs2[pp_]); qg_ = Ring(qg2[pp_]); qr = Ring(qr2[pp_])
            sq = sq2[pp_]; ssq = ssq2[pp_]; rq = rq2[pp_]; ta = ta2[pp_]; tb = tb2[pp_]
            Lt = Lt2[pp_]; cqn = cqn2[pp_]; cqT = cqT2[pp_]; qc = qc2[pp_]; qcg = qcg2[pp_]; qcr = qcr2[pp_]
            ckn = ckn2[pp_]; ckT = ckT2[pp_]; kv = kv2[pp_]; knb = knb2[pp_]; kpn = kpn2[pp_]; kpr = kpr2[pp_]
            iqf = iqf2[pp_]; iqr = iqr2[pp_]; ab = ab2[pp_]; ikr = ikr2[pp_]; ikb = ikb2[pp_]; aw = aw2[pp_]

            def rotary(src, dst, H, half, lo, coff, t, r, w):
                cs = bc(cosT[:, t, coff:coff + half].unsqueeze(1), [128, H, half])
                sn = bc(sinT[:, t, coff:coff + half].unsqueeze(1), [128, H, half])
                x1 = src[:, :, lo:lo + half]; x2 = src[:, :, lo + half:lo + 2 * half]
                ta_ = ta[:, 0:H * half].rearrange("p (h d) -> p h d", h=H); tb_ = tb[:, 0:H * half].rearrange("p (h d) -> p h d", h=H)
                P.tt("dve", ta_, x1, cs, ALU.mult, r=r + [cosT], w=[ta])
                P.tt("dve", tb_, x2, sn, ALU.mult, r=r + [sinT], w=[tb])
                P.tt("dve", dst[:, :, lo:lo + half], ta_, tb_, ALU.subtract, r=[ta, tb], w=w)
                P.tt("dve", ta_, x2, cs, ALU.mult, r=r + [cosT], w=[ta])
                P.tt("dve", tb_, x1, sn, ALU.mult, r=r + [sinT], w=[tb])
                P.tt("dve", dst[:, :, lo + half:lo + 2 * half], ta_, tb_, ALU.add, r=[ta, tb], w=w)

            def hrstd(src_view, nh, dh, r, sdt):
                sqv = sq[:, 0:nh * dh].rearrange("p (h d) -> p h d", h=nh)
                P.tt("pool", sqv, src_view, src_view, ALU.mult, r=r, w=[sq])
                P.reduce(ssq[:, 0:nh], sqv, ALU.add, r=[sq], w=[ssq])
                rstd_of(None, ssq[:, 0:nh], float(dh), nh, sdt, rq, [ssq], None)

            o = (xt.get(t), junk, ss.get(t), sd.get(t), rstd.get(t), xn.get(t))
            xT = xnT.get(t)
            sdt = sd.get(t)
            norm_tile(o, g["h1_s"], t, gc, xT, pT)
            yield
            for n, pt_, wdt in ((0, pQd, 512), (1, pKd, 512), (2, pVd, 512), (3, pL, 512), (4, pIq, 256)):
                for c in range(8):
                    P.mm(pt_[:, 0:wdt], xT[:, c, :], W[:, c, n * 512:n * 512 + wdt], c == 0, c == 7, r=[xT, Wd[c]], w=[pt_])
            v_ = vb.get(t)
            P.copy("act", qs.get(0)[:], pQd[:], r=[pQd], w=[qs.get(0)])
            P.copy("act", qs.get(1)[:], pKd[:], r=[pKd], w=[qs.get(1)])
            P.copy("act", v_[:], pVd[:], r=[pVd], w=[v_])
            P.copy("act", Lt[:], pL[:], r=[pL], w=[Lt])
            P.copy("act", iqf[:], pIq[:, 0:256], r=[pIq], w=[iqf])
            P.copy("act", iqr[:], pIq[:, 0:256], r=[pIq], w=[iqr])
            yield
            for which, gt, dst in ((0, dqg, g["qTd_s"]), (1, dkg, g["kTd_s"])):
                q_ = qs.get(which); qgg = qg_.get(which); qr_ = qr.get(which); qT_ = qTt.get(2 * t + which)
                q3 = q_[:].rearrange("p (h d) -> p h d", h=8)
                hrstd(q3, 8, 64, [q_], sdt)
                g3 = qgg[:].rearrange("p (h d) -> p h d", h=8)
                P.tt("dve", g3, q3, bc(rq[:, 0:8].unsqueeze(2), [128, 8, 64]), ALU.mult, r=[q_, rq], w=[qgg])
                P.tt("pool", g3, g3, bc(gt[:, :].unsqueeze(1), [128, 8, 64]), ALU.mult, r=[qgg, gt], w=[qgg])
                yield
                r3 = qr_[:].rearrange("p (h d) -> p h d", h=8)
                P.copy("act", qr_[:], qgg[:], r=[qgg], w=[qr_])
                rotary(g3, r3, 8, 8, 0, 16, t, [qgg], [qr_])
                yield
                for i in range(4):
                    P.tr(pT[:, i * 128:(i + 1) * 128], qr_[:, i * 128:(i + 1) * 128], ident_b[:], r=[qr_, ident_b], w=[pT])
                P.copy("act", qT_[:], pT[:, 0:512].rearrange("p (i t) -> p i t", i=4), r=[pT], w=[qT_])
                P.dma("sp", dst[:, :, t * 128:(t + 1) * 128].rearrange("i p t -> p i t"), qT_[:], r=[qT_])
                yield
            P.dma("sp", g["vd_s"][t * 128:(t + 1) * 128, :], v_[:], r=[v_])
            hrstd(Lt[:, 0:256].rearrange("p (h d) -> p h d", h=1), 1, 256, [Lt], sdt)
            P.ts("dve", cqn[:], Lt[:, 0:256], rq[:, 0:1], None, ALU.mult, None, r=[Lt, rq], w=[cqn])
            yield
            for c in range(2):
                P.tr(pT[:, c * 128:(c + 1) * 128], cqn[:, c * 128:(c + 1) * 128], ident_b[:], r=[cqn, ident_b], w=[pT])
            P.tt("dve", cqT[:], pT[:, 0:256].rearrange("p (c t) -> p c t", c=2), bc(qlc[:, :].unsqueeze(2), [128, 2, 128]),
                 ALU.mult, r=[pT, qlc], w=[cqT])
            yield
            for n0, n1 in ((0, 512), (512, 768)):
                for c in range(2):
                    P.mm(p2[:, n0:n1], cqT[:, c, :], Wuq[:, c, n0:n1], c == 0, c == 1, r=[cqT, Wuq], w=[p2])
            P.copy("act", qc[:], p2[:, 0:768], r=[p2], w=[qc])
            yield
            c3 = qc[:].rearrange("p (h d) -> p h d", h=8)
            hrstd(c3, 8, 96, [qc], sdt)
            cg3 = qcg[:].rearrange("p (h d) -> p h d", h=8)
            P.tt("dve", cg3, c3, bc(rq[:, 0:8].unsqueeze(2), [128, 8, 96]), ALU.mult, r=[qc, rq], w=[qcg])
            P.tt("pool", cg3, cg3, bc(qg96[:, :].unsqueeze(1), [128, 8, 96]), ALU.mult, r=[qcg, qg96], w=[qcg])
            yield
            P.copy("act", qcr[:], qcg[:], r=[qcg], w=[qcr])
            rotary(cg3, qcr[:].rearrange("p (h d) -> p h d", h=8), 8, 16, 64, 0, t, [qcg], [qcr])
            yield
            for h in range(8):
                P.tr(pT[0:96, h * 128:(h + 1) * 128], qcr[:, h * 96:(h + 1) * 96], ident_b[:], r=[qcr, ident_b], w=[pT])
            qcT_ = qcT.get(t)
            P.copy("act", qcT_[:], pT[0:96, :].rearrange("p (h t) -> p h t", h=8), r=[pT], w=[qcT_])
            P.dma("sp", g["qTc_s"][:, :, t * 128:(t + 1) * 128].rearrange("h p t -> p h t"), qcT_[:], r=[qcT_])
            yield
            hrstd(Lt[:, 256:384].rearrange("p (h d) -> p h d", h=1), 1, 128, [Lt], sdt)
            P.ts("dve", ckn[:], Lt[:, 256:384], rq[:, 0:1], None, ALU.mult, None, r=[Lt, rq], w=[ckn])
            yield
            P.tr(pT[:, 0:128], ckn[:], ident_b[:], r=[ckn, ident_b], w=[pT])
            P.ts("dve", ckT[:], pT[:, 0:128], kvc[:, 0:1], None, ALU.mult, None, r=[pT, kvc], w=[ckT])
            yield
            for n in range(2):
                P.mm(p2[:, n * 512:(n + 1) * 512], ckT[:], Wukv[:, n * 512:(n + 1) * 512], True, True, r=[ckT, Wukv], w=[p2])
            P.copy("act", kv[:], p2[:], r=[p2], w=[kv])
            yield
            kv3 = kv[:].rearrange("p (h d) -> p h d", h=8)
            hrstd(kv3[:, :, 0:64], 8, 64, [kv], sdt)
            P.tt("dve", knb[:], kv3[:, :, 0:64], bc(rq[:, 0:8].unsqueeze(2), [128, 8, 64]), ALU.mult, r=[kv, rq], w=[knb])
            vc_ = vcb.get(t)
            P.copy("pool", vc_[:], kv3[:, :, 64:128], r=[kv], w=[vc_])
            P.dma("sp", g["vc_s"][t * 128:(t + 1) * 128, :].rearrange("p (h d) -> p h d", h=8), vc_[:], r=[vc_])
            yield
            for h in range(8):
                P.tr(pT[0:64, h * 128:(h + 1) * 128], knb[:, h, :], ident_b[:], r=[knb, ident_b], w=[pT])
            kcT_ = kcT.get(t)
            P.ts("dve", kcT_[:], pT[0:64, :].rearrange("p (h t) -> p h t", h=8), knc[0:64, 0:1], None, ALU.mult, None,
                 r=[pT, knc], w=[kcT_])
            P.dma("sp", g["kTc_s"][:, 0:64, t * 128:(t + 1) * 128].rearrange("h p t -> p h t"), kcT_[:], r=[kcT_])
            yield
            hrstd(Lt[:, 384:416].rearrange("p (h d) -> p h d", h=1), 1, 32, [Lt], sdt)
            P.ts("dve", kpn[:], Lt[:, 384:416], rq[:, 0:1], None, ALU.mult, None, r=[Lt, rq], w=[kpn])
            P.tt("dve", kpn[:], kpn[:], kr32[:], ALU.mult, r=[kpn, kr32], w=[kpn])
            yield
            rotary(kpn[:].rearrange("p (h d) -> p h d", h=1), kpr[:].rearrange("p (h d) -> p h d", h=1), 1, 16, 0, 0, t, [kpn], [kpr])
            yield
            P.tr(pT[0:32, 0:128], kpr[:], ident_b[:], r=[kpr, ident_b], w=[pT])
            krT_ = krT.get(t)
            P.copy("act", krT_[:], pT[0:32, 0:128], r=[pT], w=[krT_])
            for h in range(8):
                P.dma("sp", g["kTc_s"][h, 64:96, t * 128:(t + 1) * 128], krT_[:], r=[krT_])
            yield
            rotary(iqf[:].rearrange("p (h d) -> p h d", h=8), iqr[:].rearrange("p (h d) -> p h d", h=8), 8, 4, 0, 24, t, [iqf], [iqr])
            yield
            P.copy("pool", ikr[:], Lt[:, 416:448], r=[Lt], w=[ikr])
            rotary(Lt[:, 416:448].rearrange("p (h d) -> p h d", h=1), ikr[:].rearrange("p (h d) -> p h d", h=1), 1, 4, 0, 24, t, [Lt], [ikr])
            P.copy("pool", ikb[:, 0:32], ikr[:], r=[ikr], w=[ikb])
            P.copy("pool", ikb[:, 32:64], ikr[:], r=[ikr], w=[ikb])
            yield
            P.tr(pT[0:64, 0:128], ikb[:], ident_b[:], r=[ikb, ident_b], w=[pT])
            ikT_ = ikT.get(t)
            P.copy("act", ikT_[:], pT[0:64, 0:128], r=[pT], w=[ikT_])
            P.dma("sp", g["ikT_s"][:, t * 128:(t + 1) * 128], ikT_[:], r=[ikT_])
            yield
            P.act(sgnT[:, t, :], Lt[:, 448:456], AF.Sign, r=[Lt], w=[sgnT])
            P.stt("dve", aw[:], Lt[:, 448:456], -1.0, Lt[:, 448:456], ALU.mult, ALU.max, r=[Lt], w=[aw])
            P.ts("dve", aw[:], aw[:], 1.0 / 16.0, None, ALU.mult, None, r=[aw], w=[aw])
            P.tt("dve", ab[:].rearrange("p (h d) -> p h d", h=8), iqr[:].rearrange("p (h d) -> p h d", h=8),
                 bc(aw[:, 0:8].unsqueeze(2), [128, 8, 32]), ALU.mult, r=[iqr, aw], w=[ab])
            yield
            for i in range(4):
                P.tr(pT[0:64, i * 128:(i + 1) * 128], ab[:, i * 64:(i + 1) * 64], ident_b[:], r=[ab, ident_b], w=[pT])
            aT_ = aTt.get(t)
            P.copy("act", aT_[:], pT[0:64, 0:512].rearrange("p (i t) -> p i t", i=4), r=[pT], w=[aT_])
            P.dma("sp", g["aT_s"][:, :, t * 128:(t + 1) * 128].rearrange("i p t -> p i t"), aT_[:], r=[aT_])

        g["run_rolling"](tile_gen, NT, 2)
    P.barrier()
    stop = g["stop"]
    attention = g["attention"]; phase_CD = g["phase_CD"]
    if stop == "a1":
        return
    attention("mla", g["qTc_s"], g["kTc_s"], g["vc_s"], 0, 96.0 ** -0.5)
    if stop == "mla":
        return
    attention("dsa", g["qTd_s"], g["kTd_s"], g["vd_s"], 512, 0.125)
    if stop == "dsa":
        return
    phase_CD(1, g["h1_s"], g["out_d"], g["w_out1"])


phase_A1_holder[0] = _phase_A1
```
